# Optimizing a Trainium2 kernel written in Bass

```python
import math
import jax, jax.numpy as jnp
from jax import lax
import numpy as np

D_MODEL = 1024
BATCH = 8
SEQ = 8192
DEPTH = 4

HEAD_DIM = 64
DIFF_HEADS = 4
NSA_Q_HEADS = 8
NSA_KV_HEADS = 2
NSA_GROUP = NSA_Q_HEADS // NSA_KV_HEADS
CMP_BLOCK = 32
CMP_STRIDE = 16
CMP_HIDDEN = 256
SEL_BLOCK = 64
SEL_TOP = 16
SEL_FORCED_SCORE = 1.0e4
WINDOW = 512
Q_BLOCK = 128
CONV_WIDTH = 31
N_EXPERTS = 32
TOP_K = 4
D_EXPERT = D_MODEL
SWIGLU_ALPHA = 1.702
SWIGLU_LIMIT = 7.0
MOE_BLOCK = 256
NORM_EPS = 1e-6

DIFF_QK = DIFF_HEADS * 2 * HEAD_DIM
DIFF_V = DIFF_HEADS * 2 * HEAD_DIM
NSA_Q = NSA_Q_HEADS * HEAD_DIM
NSA_KV = NSA_KV_HEADS * HEAD_DIM
NSA_GATES = NSA_Q_HEADS * 3
IN_SPLITS = (DIFF_QK, DIFF_QK, DIFF_V, NSA_Q, 6 * NSA_KV, NSA_GATES)
W_IN = sum(IN_SPLITS)
ATTN_OUT = DIFF_V + NSA_Q

kernel_name = "hybrid_diffattn_nsa_conformer_moe_trunk"


def rms_norm(x, g):
    xf = x.astype(jnp.float32)
    y = xf * lax.rsqrt(jnp.mean(xf * xf, axis=-1, keepdims=True) + NORM_EPS)
    return (y * g.astype(jnp.float32)).astype(x.dtype)


def split_cols(z, sizes):
    return jnp.split(z, np.cumsum(sizes)[:-1].tolist(), axis=-1)


def masked_softmax(s, mask):
    s = jnp.where(mask, s, -jnp.inf)
    m = jnp.max(s, axis=-1, keepdims=True)
    m = jnp.where(jnp.isfinite(m), m, 0.0)
    e = jnp.exp(s - m)
    return e / jnp.maximum(jnp.sum(e, axis=-1, keepdims=True), 1e-30)


def over_query_blocks(fn, seq_len):
    starts = jnp.arange(seq_len // Q_BLOCK, dtype=jnp.int32) * Q_BLOCK
    out = lax.map(fn, starts)
    out = jnp.moveaxis(out, 0, 1)
    return out.reshape((out.shape[0], seq_len) + out.shape[3:])


def diff_attention(q, k, v, qk_gain, lam_vecs, subln_g, lam_init):
    B, S = q.shape[:2]
    q = rms_norm(q, qk_gain[0])
    k = rms_norm(k, qk_gain[1])
    lv = lam_vecs.astype(jnp.float32)
    lam = jnp.exp(jnp.sum(lv[0] * lv[1])) - jnp.exp(jnp.sum(lv[2] * lv[3])) + lam_init
    scale = HEAD_DIM ** -0.5
    kpos = jnp.arange(S)

    def block(qs):
        qb = lax.dynamic_slice_in_dim(q, qs, Q_BLOCK, axis=1)
        s = jnp.einsum('bqhmd,bkhmd->bhmqk', qb, k, preferred_element_type=jnp.float32) * scale
        t = qs + jnp.arange(Q_BLOCK)
        p = masked_softmax(s, kpos[None, :] <= t[:, None])
        a = p[:, :, 0] - lam * p[:, :, 1]
        return jnp.einsum('bhqk,bkhe->bqhe', a.astype(v.dtype), v)

    o = over_query_blocks(block, S)
    o = rms_norm(o, subln_g) * (1.0 - lam_init)
    return o.reshape(B, S, DIFF_HEADS * 2 * HEAD_DIM)


def nsa_attention(q, k_cmp, v_cmp, k_sel, v_sel, k_win, v_win, gates,
                  q_gain, k_gain, cmp_pos, cmp_w1, cmp_w2):
    B, S = q.shape[:2]
    q = rms_norm(q, q_gain)
    scale = HEAD_DIM ** -0.5

    n_cmp = (S - CMP_BLOCK) // CMP_STRIDE + 1
    blk_idx = np.arange(n_cmp)[:, None] * CMP_STRIDE + np.arange(CMP_BLOCK)[None, :]

    def compress(z, j):
        zb = z[:, blk_idx] + cmp_pos[j][None, None, :, None, :]
        zb = jnp.moveaxis(zb, 3, 2).reshape(B, n_cmp, NSA_KV_HEADS, CMP_BLOCK * HEAD_DIM)
        return jax.nn.gelu(zb @ cmp_w1[j]) @ cmp_w2[j]

    kc = rms_norm(compress(k_cmp, 0), k_gain[0])
    vc = compress(v_cmp, 1)
    cmp_end = jnp.asarray(blk_idx[:, -1])

    n_sel = S // SEL_BLOCK
    n_top = min(SEL_TOP, n_sel)
    ks = rms_norm(k_sel, k_gain[1]).reshape(B, n_sel, SEL_BLOCK, NSA_KV_HEADS, HEAD_DIM).transpose(0, 3, 1, 2, 4)
    vs = v_sel.reshape(B, n_sel, SEL_BLOCK, NSA_KV_HEADS, HEAD_DIM).transpose(0, 3, 1, 2, 4)
    c_start = blk_idx[:, 0]
    s_start = np.arange(n_sel) * SEL_BLOCK
    overlap = jnp.asarray(((c_start[:, None] < s_start[None, :] + SEL_BLOCK)
                           & (c_start[:, None] + CMP_BLOCK > s_start[None, :])).astype(np.float32))

    pad = ((0, 0), (WINDOW, 0), (0, 0), (0, 0))
    kw = jnp.pad(rms_norm(k_win, k_gain[2]), pad)
    vw = jnp.pad(v_win, pad)

    qg = q.reshape(B, S, NSA_KV_HEADS, NSA_GROUP, HEAD_DIM)
    bi = jnp.arange(B)[:, None, None, None]
    hi = jnp.arange(NSA_KV_HEADS)[None, :, None, None]
    sel_off = jnp.arange(SEL_BLOCK)
    win_off = jnp.arange(Q_BLOCK + WINDOW) - WINDOW
    jsel = jnp.arange(n_sel)

    def block(qs):
        qb = lax.dynamic_slice_in_dim(qg, qs, Q_BLOCK, axis=1)
        t = qs + jnp.arange(Q_BLOCK)
        s_c = jnp.einsum('bqkgd,bckd->bkgqc', qb, kc, preferred_element_type=jnp.float32) * scale
        p_c = masked_softmax(s_c, cmp_end[None, :] <= t[:, None])
        o_c = jnp.einsum('bkgqc,bckd->bqkgd', p_c.astype(vc.dtype), vc)
        imp = jnp.einsum('bkgqc,cn->bkqn', p_c, overlap)
        cur = (t // SEL_BLOCK)[:, None]
        forced = (jsel[None, :] == 0) | (jsel[None, :] == cur) | (jsel[None, :] == cur - 1)
        score = jnp.where(forced, SEL_FORCED_SCORE, jnp.where(jsel[None, :] <= cur, imp, -jnp.inf))
        _, top = lax.top_k(score, n_top)
        kg = ks[bi, hi, top]
        vg = vs[bi, hi, top]
        s_s = jnp.einsum('bqkgd,bkqnld->bkgqnl', qb, kg, preferred_element_type=jnp.float32) * scale
        pos = top[..., None] * SEL_BLOCK + sel_off
        m_s = (pos <= t[None, None, :, None, None]).reshape(B, NSA_KV_HEADS, 1, Q_BLOCK, n_top * SEL_BLOCK)
        p_s = masked_softmax(s_s.reshape(B, NSA_KV_HEADS, NSA_GROUP, Q_BLOCK, n_top * SEL_BLOCK), m_s)
        p_s = p_s.reshape(B, NSA_KV_HEADS, NSA_GROUP, Q_BLOCK, n_top, SEL_BLOCK)
        o_s = jnp.einsum('bkgqnl,bkqnld->bqkgd', p_s.astype(vg.dtype), vg)
        kwb = lax.dynamic_slice_in_dim(kw, qs, Q_BLOCK + WINDOW, axis=1)
        vwb = lax.dynamic_slice_in_dim(vw, qs, Q_BLOCK + WINDOW, axis=1)
        kpos = qs + win_off
        dist = t[:, None] - kpos[None, :]
        m_w = (kpos[None, :] >= 0) & (dist >= 0) & (dist < WINDOW)
        s_w = jnp.einsum('bqkgd,bskd->bkgqs', qb, kwb, preferred_element_type=jnp.float32) * scale
        p_w = masked_softmax(s_w, m_w)
        o_w = jnp.einsum('bkgqs,bskd->bqkgd', p_w.astype(vwb.dtype), vwb)
        return jnp.stack([o_c, o_s, o_w], axis=-1)

    o = over_query_blocks(block, S)
    g = jax.nn.sigmoid(gates.astype(jnp.float32)).reshape(B, S, NSA_KV_HEADS, NSA_GROUP, 1, 3)
    out = jnp.sum(o * g.astype(o.dtype), axis=-1)
    return out.reshape(B, S, NSA_Q)


def attention_mixer(h, w_in, w_out, diff_qk_gain, diff_lambda, diff_subln,
                    nsa_q_gain, nsa_k_gain, cmp_pos, cmp_w1, cmp_w2, lam_init):
    B, S, _ = h.shape
    z = h @ w_in
    dq, dk, dv, nq, nkv, ng = split_cols(z, IN_SPLITS)
    o_diff = diff_attention(dq.reshape(B, S, DIFF_HEADS, 2, HEAD_DIM),
                            dk.reshape(B, S, DIFF_HEADS, 2, HEAD_DIM),
                            dv.reshape(B, S, DIFF_HEADS, 2 * HEAD_DIM),
                            diff_qk_gain, diff_lambda, diff_subln, lam_init)
    kv = nkv.reshape(B, S, 6, NSA_KV_HEADS, HEAD_DIM)
    o_nsa = nsa_attention(nq.reshape(B, S, NSA_Q_HEADS, HEAD_DIM),
                          kv[:, :, 0], kv[:, :, 1], kv[:, :, 2], kv[:, :, 3], kv[:, :, 4], kv[:, :, 5],
                          ng.reshape(B, S, NSA_Q_HEADS, 3),
                          nsa_q_gain, nsa_k_gain, cmp_pos, cmp_w1, cmp_w2)
    return jnp.concatenate([o_diff, o_nsa], axis=-1) @ w_out


def conv_module(h, pw1_w, pw1_b, dw_w, dw_b, ln_g, ln_b, pw2_w, pw2_b):
    u = h @ pw1_w + pw1_b
    a, b = jnp.split(u, 2, axis=-1)
    u = a * jax.nn.sigmoid(b)
    u = lax.conv_general_dilated(u, dw_w, window_strides=(1,), padding=[(CONV_WIDTH - 1, 0)],
                                 dimension_numbers=("NWC", "WIO", "NWC"),
                                 feature_group_count=u.shape[-1]) + dw_b
    uf = u.astype(jnp.float32)
    mu = jnp.mean(uf, axis=-1, keepdims=True)
    var = jnp.mean(jnp.square(uf - mu), axis=-1, keepdims=True)
    uf = (uf - mu) * lax.rsqrt(var + NORM_EPS) * ln_g.astype(jnp.float32) + ln_b.astype(jnp.float32)
    u = jax.nn.silu(uf).astype(h.dtype)
    return u @ pw2_w + pw2_b


def clamped_swiglu(u):
    glu, lin = u[..., ::2], u[..., 1::2]
    glu = jnp.minimum(glu, SWIGLU_LIMIT)
    lin = jnp.clip(lin, -SWIGLU_LIMIT, SWIGLU_LIMIT)
    return glu * jax.nn.sigmoid(SWIGLU_ALPHA * glu) * (lin + 1.0)


def moe_ffn(h, router_w, router_b, w1, b1, w2, b2):
    B, S, D = h.shape
    xt = h.reshape(-1, D)
    T = xt.shape[0]
    logits = (xt @ router_w + router_b).astype(jnp.float32)
    top_val, top_idx = lax.top_k(logits, TOP_K)
    weights = jax.nn.softmax(top_val, axis=-1)
    n_assign = T * TOP_K
    flat_e = top_idx.reshape(-1)
    order = jnp.argsort(flat_e)
    sorted_e = flat_e[order]
    counts = jnp.bincount(flat_e, length=N_EXPERTS)
    padded = (counts + MOE_BLOCK - 1) // MOE_BLOCK * MOE_BLOCK
    start = jnp.cumsum(counts) - counts
    pad_end = jnp.cumsum(padded)
    pad_start = pad_end - padded
    dest = pad_start[sorted_e] + jnp.arange(n_assign, dtype=jnp.int32) - start[sorted_e]
    n_blocks = -(-n_assign // MOE_BLOCK) + N_EXPERTS
    n_slots = n_blocks * MOE_BLOCK
    slot_token = jnp.zeros((n_slots,), jnp.int32).at[dest].set((order // TOP_K).astype(jnp.int32))
    slot_w = jnp.zeros((n_slots,), jnp.float32).at[dest].set(weights.reshape(-1)[order])
    block_expert = jnp.minimum(
        jnp.searchsorted(pad_end, jnp.arange(n_blocks, dtype=jnp.int32) * MOE_BLOCK, side='right'),
        N_EXPERTS - 1)
    xs = xt[slot_token].reshape(n_blocks, MOE_BLOCK, D)

    def expert_block(args):
        xb, wb, e = args
        u = clamped_swiglu(xb @ w1[e] + b1[e])
        return (u @ w2[e] + b2[e]) * wb.astype(xb.dtype)[:, None]

    ys = lax.map(expert_block, (xs, slot_w.reshape(n_blocks, MOE_BLOCK), block_expert))
    out = jnp.zeros((T, D), ys.dtype).at[slot_token].add(ys.reshape(-1, D))
    return out.reshape(B, S, D)


def setup_inputs(seed: int = 0) -> dict:
    key = jax.random.key(seed)
    keys = jax.random.split(key, 40)
    counter = [0]
    n_even = (DEPTH + 1) // 2
    n_odd = DEPTH // 2
    D, E, F = D_MODEL, N_EXPERTS, D_EXPERT

    def nrm(shape, s):
        k = keys[counter[0]]
        counter[0] += 1
        return s * jax.random.normal(k, shape, jnp.float32)

    def gain(shape):
        return 1.0 + nrm(shape, 0.05)

    return {
        "x": nrm((BATCH, SEQ, D), 1.0),
        "c": nrm((BATCH, D), 1.0),
        "mod_w": nrm((DEPTH, D, 6 * D), 0.3 * D ** -0.5),
        "mod_b": nrm((DEPTH, 6 * D), 0.02),
        "norm_mix": gain((DEPTH, D)),
        "norm_ffn": gain((DEPTH, D)),
        "attn_w_in": nrm((n_even, D, W_IN), D ** -0.5),
        "attn_w_out": nrm((n_even, ATTN_OUT, D), ATTN_OUT ** -0.5),
        "diff_qk_gain": gain((n_even, 2, HEAD_DIM)),
        "diff_lambda": nrm((n_even, 4, HEAD_DIM), 0.1),
        "diff_subln": gain((n_even, 2 * HEAD_DIM)),
        "nsa_q_gain": gain((n_even, HEAD_DIM)),
        "nsa_k_gain": gain((n_even, 3, HEAD_DIM)),
        "nsa_cmp_pos": nrm((n_even, 2, CMP_BLOCK, HEAD_DIM), 0.1),
        "nsa_cmp_w1": nrm((n_even, 2, CMP_BLOCK * HEAD_DIM, CMP_HIDDEN), (CMP_BLOCK * HEAD_DIM) ** -0.5),
        "nsa_cmp_w2": nrm((n_even, 2, CMP_HIDDEN, HEAD_DIM), CMP_HIDDEN ** -0.5),
        "conv_pw1_w": nrm((n_odd, D, 2 * D), D ** -0.5),
        "conv_pw1_b": nrm((n_odd, 2 * D), 0.02),
        "conv_dw_w": nrm((n_odd, CONV_WIDTH, 1, D), CONV_WIDTH ** -0.5),
        "conv_dw_b": nrm((n_odd, D), 0.02),
        "conv_ln_g": gain((n_odd, D)),
        "conv_ln_b": nrm((n_odd, D), 0.02),
        "conv_pw2_w": nrm((n_odd, D, D), D ** -0.5),
        "conv_pw2_b": nrm((n_odd, D), 0.02),
        "router_w": nrm((DEPTH, D, E), D ** -0.5),
        "router_b": nrm((DEPTH, E), 0.01),
        "moe_w1": nrm((DEPTH, E, D, 2 * F), D ** -0.5),
        "moe_b1": nrm((DEPTH, E, 2 * F), 0.02),
        "moe_w2": nrm((DEPTH, E, F, D), F ** -0.5),
        "moe_b2": nrm((DEPTH, E, D), 0.02),
    }


def reference(x, c, mod_w, mod_b, norm_mix, norm_ffn, attn_w_in, attn_w_out,
              diff_qk_gain, diff_lambda, diff_subln, nsa_q_gain, nsa_k_gain,
              nsa_cmp_pos, nsa_cmp_w1, nsa_cmp_w2,
              conv_pw1_w, conv_pw1_b, conv_dw_w, conv_dw_b, conv_ln_g, conv_ln_b,
              conv_pw2_w, conv_pw2_b,
              router_w, router_b, moe_w1, moe_b1, moe_w2, moe_b2):
    cond = jax.nn.silu(c)
    for i in range(DEPTH):
        mod = (cond @ mod_w[i] + mod_b[i])[:, None, :]
        sh1, sc1, g1, sh2, sc2, g2 = jnp.split(mod, 6, axis=-1)
        h = rms_norm(x, norm_mix[i]) * (1.0 + sc1) + sh1
        j = i // 2
        if i % 2 == 0:
            lam_init = 0.8 - 0.6 * math.exp(-0.3 * i)
            y = attention_mixer(h, attn_w_in[j], attn_w_out[j], diff_qk_gain[j], diff_lambda[j],
                                diff_subln[j], nsa_q_gain[j], nsa_k_gain[j],
                                nsa_cmp_pos[j], nsa_cmp_w1[j], nsa_cmp_w2[j], lam_init)
        else:
            y = conv_module(h, conv_pw1_w[j], conv_pw1_b[j], conv_dw_w[j], conv_dw_b[j],
                            conv_ln_g[j], conv_ln_b[j], conv_pw2_w[j], conv_pw2_b[j])
        x = x + g1 * y
        h = rms_norm(x, norm_ffn[i]) * (1.0 + sc2) + sh2
        x = x + g2 * moe_ffn(h, router_w[i], router_b[i], moe_w1[i], moe_b1[i], moe_w2[i], moe_b2[i])
    return x
```

```python
import math
from contextlib import ExitStack

import numpy as np
import ml_dtypes
import concourse.bass as bass
import concourse.mybir as mybir
from concourse.bass_utils import run_bass_kernel_spmd

F32 = mybir.dt.float32
BF16 = mybir.dt.bfloat16
I32 = mybir.dt.int32
AF = mybir.ActivationFunctionType
ALU = mybir.AluOpType
AX = mybir.AxisListType

S = 8192
D = 1024
NT = S // 128
DEPTH = 4
NE = 32
EPS = 1e-6
W_IN = 2840
BIG = 30000.0
SHARD_EXPERTS = False


class Em:
    ENGS = ("pe", "act", "dve", "pool", "sp")
    EPOCH = 60000
    RING = 12

    def __init__(self, nc, es):
        self.nc = nc
        self.es = es
        self.eo = dict(pe=nc.tensor, act=nc.scalar, dve=nc.vector, pool=nc.gpsimd, sp=nc.sync)
        self.seq = {e: 0 for e in self.ENGS}
        self.cursem = {e: None for e in self.ENGS}
        self.curval = {e: 0 for e in self.ENGS}
        self.waited = {e: {f: 0 for f in self.ENGS} for e in self.ENGS}
        self.waited_dma = {e: {} for e in self.ENGS}
        self.last_w = {}
        self.readers = {}
        self.ring = {}
        self.dma_n = {}
        self.nsem = 0
        self.ninst = 0
        self.cc = []
        self.ccsem = None

    def _newsem(self, tag):
        self.nsem += 1
        return self.es.enter_context(self.nc.semaphore(f"{tag}{self.nsem}"))

    def _wait(self, eng, dep):
        if dep[0] == "c":
            _, f, seq, sem, val = dep
            if self.waited[eng][f] >= seq:
                return
            self.eo[eng].wait_ge(sem, val)
            self.waited[eng][f] = seq
        else:
            _, sid, sem, val = dep
            if self.waited_dma[eng].get(sid, 0) >= val:
                return
            self.eo[eng].wait_ge(sem, val)
            self.waited_dma[eng][sid] = val

    def _deps(self, eng, reads, writes, is_dma):
        for k in reads:
            d = self.last_w.get(k)
            if d is not None:
                self._wait(eng, d)
        for k in writes:
            d = self.last_w.get(k)
            if d is not None and (is_dma or d[0] == "d" or d[1] != eng):
                self._wait(eng, d)
            r = self.readers.get(k)
            if r:
                for f, dd in r[0].items():
                    if is_dma or f != eng:
                        self._wait(eng, dd)
                for dd in r[1]:
                    self._wait(eng, dd)

    def _record(self, me, reads, writes):
        for k in writes:
            self.last_w[k] = me
            self.readers[k] = ({}, [])
        for k in reads:
            r = self.readers.get(k)
            if r is None:
                r = ({}, [])
                self.readers[k] = r
            if me[0] == "c":
                r[0][me[1]] = me
            else:
                r[1].append(me)

    def op(self, eng, fn, reads=(), writes=()):
        self._deps(eng, reads, writes, False)
        ins = fn(self.eo[eng])
        if self.cursem[eng] is None or self.curval[eng] >= self.EPOCH:
            self.cursem[eng] = self._newsem("c" + eng)
            self.curval[eng] = 0
        self.curval[eng] += 1
        self.seq[eng] += 1
        ins.then_inc(self.cursem[eng], 1)
        me = ("c", eng, self.seq[eng], self.cursem[eng], self.curval[eng])
        self._record(me, reads, writes)
        self.ninst += 1
        return me

    def dma(self, q, out, in_, reads=(), writes=(), **kw):
        self._deps(q, reads, writes, True)
        if q not in self.ring:
            self.ring[q] = [self._newsem("d" + q) for _ in range(self.RING)]
            self.dma_n[q] = 0
        n = self.dma_n[q]
        self.dma_n[q] = n + 1
        slot = n % self.RING
        sem = self.ring[q][slot]
        sid = (q, slot)
        prev = 16 * (n // self.RING)
        if prev > 0:
            self._wait(q, ("d", sid, sem, prev))
        self.eo[q].dma_start(out=out, in_=in_, **kw).then_inc(sem, 16)
        me = ("d", sid, sem, prev + 16)
        self._record(me, reads, writes)
        self.ninst += 1
        return me

    def coll_allgather(self, in_ap, out_ap, n, reads=(), writes=()):
        q = "pool"
        self._deps(q, reads, writes, True)
        if self.ccsem is None:
            self.ccsem = self._newsem("cc")
            self.ccn = 0
        ins = self.nc.gpsimd.collective_compute("AllGather", ALU.bypass, replica_groups=[list(range(n))],
                                                ins=[in_ap], outs=[out_ap])
        ins.then_inc(self.ccsem)
        self.ccn += 1
        me = ("d", ("cc", 0), self.ccsem, self.ccn)
        self._record(me, reads, writes)
        self.cc = [me]
        return me

    def barrier(self):
        for eng in self.ENGS:
            for me in self.cc:
                self._wait(eng, me)
            for q, sems in self.ring.items():
                n = self.dma_n[q]
                for slot, sem in enumerate(sems):
                    cnt = (n - slot + self.RING - 1) // self.RING
                    if cnt > 0:
                        self._wait(eng, ("d", (q, slot), sem, 16 * cnt))
            for e in ("pe", "act", "dve", "pool"):
                if e != eng and self.cursem[e] is not None:
                    self._wait(eng, ("c", e, self.seq[e], self.cursem[e], self.curval[e]))

    def finish(self):
        for me in self.cc:
            self._wait("sp", me)
        for q, sems in self.ring.items():
            n = self.dma_n[q]
            for slot, sem in enumerate(sems):
                cnt = (n - slot + self.RING - 1) // self.RING
                if cnt > 0:
                    self._wait("sp", ("d", (q, slot), sem, 16 * cnt))
        for e in ("pe", "act", "dve", "pool"):
            if self.cursem[e] is not None:
                self._wait("sp", ("c", e, self.seq[e], self.cursem[e], self.curval[e]))


class LSel:
    def __init__(self, ap, sel):
        self.ap, self.sel = ap, sel

    def _m(self, k):
        if isinstance(k, slice):
            return slice(k.start - self.sel, k.stop - self.sel, k.step)
        return k - self.sel

    def __getitem__(self, key):
        if isinstance(key, tuple):
            return self.ap[(self._m(key[0]),) + tuple(key[1:])]
        return self.ap[self._m(key)]


L_LEAD = ("mod_w", "mod_b", "norm_mix", "norm_ffn", "router_w", "router_b", "moe_w1", "moe_b1", "moe_w2", "moe_b2")
J_LEAD = ("attn_w_in", "attn_w_out", "diff_qk_gain", "diff_lambda", "diff_subln", "nsa_q_gain", "nsa_k_gain",
          "nsa_cmp_pos", "nsa_cmp_w1", "nsa_cmp_w2", "conv_pw1_w", "conv_pw1_b", "conv_dw_w", "conv_dw_b",
          "conv_ln_g", "conv_ln_b", "conv_pw2_w", "conv_pw2_b")
ATTN_IN = ("attn_w_in", "attn_w_out", "diff_qk_gain", "diff_lambda", "diff_subln", "nsa_q_gain", "nsa_k_gain",
           "nsa_cmp_pos", "nsa_cmp_w1", "nsa_cmp_w2", "trimask", "cmpmask", "winmask", "ebig", "ovl", "selkeep", "seladd")
CONV_IN = ("conv_pw1_w", "conv_pw1_b", "conv_dw_w", "conv_dw_b", "conv_ln_g", "conv_ln_b", "conv_pw2_w", "conv_pw2_b")
FFN_IN = ("router_w", "router_b", "moe_w1", "moe_b1", "moe_w2", "moe_b2")


class Prog:
    def __init__(self, plan, dbg=(), opts=None, ncores=8):
        self.ncores = ncores
        self.nr = 8 if (ncores == 8 and SHARD_EXPERTS) else 1
        self.epc = NE // self.nr
        self.plan = plan
        self.dbg = dbg
        self.opts = opts or {}
        self.nc = bass.Bass("TRN2", target_bir_lowering=False)
        self.es = ExitStack()
        self.em = Em(self.nc, self.es)
        self.din = {}
        self.lsel = {}
        self.uid = 0

    def inp(self, name, shape, dt=F32):
        kinds = {k for k, _ in self.plan}
        single = len({l_ for _, l_ in self.plan}) == 1
        if single:
            l = self.plan[0][1]
            used = True
            if name in ATTN_IN:
                used = "mix" in kinds and l % 2 == 0
            elif name in CONV_IN:
                used = "mix" in kinds and l % 2 == 1
            elif name in FFN_IN:
                used = "ffn" in kinds
            if not used:
                return None
            if name in L_LEAD or name in J_LEAD:
                sel = l if name in L_LEAD else l // 2
                t = self.nc.dram_tensor(name, [1] + list(shape)[1:], dt, kind="ExternalInput").ap()
                self.din[name] = t
                self.lsel[name] = sel
                return LSel(t, sel)
        t = self.nc.dram_tensor(name, list(shape), dt, kind="ExternalInput").ap()
        self.din[name] = t
        return t

    def scratch(self, name, shape, dt):
        kind = "ExternalOutput" if name in self.dbg else "Internal"
        return self.nc.dram_tensor(name, list(shape), dt, kind=kind).ap()

    def scope(self):
        prog = self

        class _Scope(ExitStack):
            def __exit__(self, *a):
                if a[0] is None:
                    prog.em.barrier()
                return super().__exit__(*a)
        return _Scope()

    def dump(self, name, ap, shape, dt, keys):
        if name not in self.dbg:
            return
        t = self.nc.dram_tensor(name, list(shape), dt, kind="ExternalOutput").ap()
        self.em.dma("sp", t, ap, reads=keys)

    def sb(self, es, name, shape, dt=F32):
        self.nsb = getattr(self, "nsb", 0) + 1
        return es.enter_context(self.nc.sbuf_tensor(f"{name}_{self.nsb}", list(shape), dt))

    def declare(self):
        i = self.inp
        self.x = i("x", [S, D])
        self.c = i("c", [1, D])
        self.mod_w = i("mod_w", [DEPTH, D, 6 * D])
        self.mod_b = i("mod_b", [DEPTH, 6 * D])
        self.norm_mix = i("norm_mix", [DEPTH, D])
        self.norm_ffn = i("norm_ffn", [DEPTH, D])
        self.attn_w_in = i("attn_w_in", [2, D, W_IN])
        self.attn_w_out = i("attn_w_out", [2, D, D])
        self.diff_qk_gain = i("diff_qk_gain", [2, 2, 64])
        self.diff_lambda = i("diff_lambda", [2, 4, 64])
        self.diff_subln = i("diff_subln", [2, 128])
        self.nsa_q_gain = i("nsa_q_gain", [2, 64])
        self.nsa_k_gain = i("nsa_k_gain", [2, 3, 64])
        self.nsa_cmp_pos = i("nsa_cmp_pos", [2, 2, 32, 64])
        self.nsa_cmp_w1 = i("nsa_cmp_w1", [2, 2, 2048, 256])
        self.nsa_cmp_w2 = i("nsa_cmp_w2", [2, 2, 256, 64])
        self.conv_pw1_w = i("conv_pw1_w", [2, D, 2 * D])
        self.conv_pw1_b = i("conv_pw1_b", [2, 2 * D])
        self.conv_dw_w = i("conv_dw_w", [2, 31, D])
        self.conv_dw_b = i("conv_dw_b", [2, D])
        self.conv_ln_g = i("conv_ln_g", [2, D])
        self.conv_ln_b = i("conv_ln_b", [2, D])
        self.conv_pw2_w = i("conv_pw2_w", [2, D, D])
        self.conv_pw2_b = i("conv_pw2_b", [2, D])
        self.router_w = i("router_w", [DEPTH, D, NE])
        self.router_b = i("router_b", [DEPTH, NE])
        self.moe_w1 = i("moe_w1", [DEPTH, self.epc, D, 2 * D])
        self.moe_b1 = i("moe_b1", [DEPTH, NE, 2 * D])
        self.moe_w2 = i("moe_w2", [DEPTH, self.epc, D, D])
        self.moe_b2 = i("moe_b2", [DEPTH, NE, D])
        self.ident_in = i("ident", [128, 128])
        i("trimask", [128, 128], BF16)
        i("cmpmask", [5, 128, 512], BF16)
        i("winmask", [8, 128, 512], BF16)
        i("ebig", [128, S], BF16)
        i("ovl", [128, 4, 128], BF16)
        i("selkeep", [64, 128, 128], F32)
        i("seladd", [64, 128, 128], F32)
        self.y = self.nc.dram_tensor("y", [S, D], F32, kind="ExternalOutput").ap()
        self.w1sh = [self.scratch(f"w1sh{l}", [self.epc * 64, 128, 256], BF16) for l in range(DEPTH)]
        self.w2sh = [self.scratch(f"w2sh{l}", [self.epc * 32, 128, 256], BF16) for l in range(DEPTH)]
        if self.nr > 1:
            self.w1g = [self.scratch(f"w1g{l}", [self.epc * 64, self.nr * 128, 256], BF16) for l in range(DEPTH)]
            self.w2g = [self.scratch(f"w2g{l}", [self.epc * 32, self.nr * 128, 256], BF16) for l in range(DEPTH)]
        else:
            self.w1g, self.w2g = self.w1sh, self.w2sh

    def build(self):
        nc, em = self.nc, self.em
        self.declare()
        with self.es:
            es = self.es
            self.ident = self.sb(es, "identf", [128, 128], F32)
            self.identb = self.sb(es, "identb", [128, 128], BF16)
            self.onesf = self.sb(es, "onesf", [128, 128], F32)
            self.condT = self.sb(es, "condT", [128, 8], F32)
            self.grow = self.sb(es, "grow", [1, 2, D], F32)
            self.tstage = self.sb(es, "tstage", [32, 128], F32)
            self.modT = self.sb(es, "modT", [128, 48], F32)
            self.gbc = self.sb(es, "gbc", [128, D], F32)
            self.aT = self.sb(es, "aT", [128, 8], F32)
            self.nrmT = self.sb(es, "nrmT", [128, 8], F32)
            self.ps = [es.enter_context(nc.psum_tensor(f"ps{b}", [128, 512], F32)) for b in range(8)]
            em.dma("sp", self.ident[:], self.ident_in[:, :], writes=["ident"])
            em.op("dve", lambda e: e.tensor_copy(out=self.identb[:], in_=self.ident[:]),
                  reads=["ident"], writes=["identb"])
            em.op("dve", lambda e: e.memset(self.onesf[:], 1.0), writes=["onesf"])
            self.epsT = self.sb(es, "epsT", [128, 1], F32)
            em.op("dve", lambda e: e.memset(self.epsT[:], EPS), writes=["epsT"])
            self.load_T(self.condT[:], self.c.rearrange("o (k p) -> (o k) p", p=128), 8, "condT")
            with self.scope() as es2:
                tmp = self.sb(es2, "condtmp", [128, 8], F32)
                em.op("act", lambda e: e.activation(out=tmp[:], in_=self.condT[:], func=AF.Sigmoid),
                      reads=["condT"], writes=["condtmp"])
                em.op("dve", lambda e: e.tensor_tensor(out=self.condT[:], in0=self.condT[:], in1=tmp[:], op=ALU.mult),
                      reads=["condT", "condtmp"], writes=["condT"])
                if any(p[0] == "ffn" for p in self.plan):
                    self.cast_moe_weights(sorted({p[1] for p in self.plan if p[0] == "ffn"}))
            first = True
            cur_layer = None
            for kind, l in self.plan:
                if cur_layer != l:
                    self.layer_mod(l)
                    cur_layer = l
                src = self.x if first else self.y
                first = False
                if kind == "mix":
                    if l % 2 == 1:
                        self.conv_layer(l, src)
                    else:
                        self.attn_layer(l, src)
                else:
                    self.moe_layer(l, src)
            em.finish()
        return nc

    def load_T(self, dst, src_rows, n, key):
        em = self.em
        em.dma("sp", self.tstage[0:n, 0:128], src_rows, writes=["tstage"])
        em.op("pe", lambda e: e.transpose(out=self.ps[7][:, 0:n], in_=self.tstage[0:n, 0:128],
                                          identity=self.ident[0:n, 0:n]), reads=["tstage", "ident"], writes=["ps7"])
        em.op("dve", lambda e: e.tensor_copy(out=dst, in_=self.ps[7][:, 0:n]), reads=["ps7"], writes=[key])

    def layer_mod(self, l):
        nc, em = self.nc, self.em
        with self.scope() as es:
            wt = [self.sb(es, f"modw{i}", [128, 8, 512], F32) for i in range(2)]
            mb = self.sb(es, "modb", [1, 6 * D], F32)
            self.modrow = self.sb(es, "modrow", [1, 6 * D], F32)
            em.dma("sp", mb[:], self.mod_b[l:l + 1, :], writes=["modb"])
            mw = self.mod_w[l].rearrange("(k p) n -> p k n", p=128)
            for n in range(12):
                w = wt[n % 2]
                em.dma("sp", w[:], mw[:, :, n * 512:(n + 1) * 512], writes=[f"modw{n % 2}"])
                pst = self.ps[n % 2]

                def mm(e, w=w, pst=pst):
                    for k in range(8):
                        r = e.matmul(pst[0:1, :], lhsT=self.condT[:, k:k + 1], rhs=w[:, k, :],
                                     start=(k == 0), stop=(k == 7))
                    return r
                em.op("pe", mm, reads=["condT", f"modw{n % 2}"], writes=[f"ps{n % 2}"])
                em.op("dve", lambda e, n=n, pst=pst: e.tensor_tensor(
                    out=self.modrow[0:1, n * 512:(n + 1) * 512], in0=pst[0:1, :],
                    in1=mb[0:1, n * 512:(n + 1) * 512], op=ALU.add),
                    reads=[f"ps{n % 2}", "modb"], writes=["modrow"])
            def mmT(e):
                for j in range(48):
                    r = e.matmul(self.ps[2][:, j:j + 1], lhsT=self.modrow[0:1, j * 128:(j + 1) * 128],
                                 rhs=self.onesf[0:1, 0:1], start=True, stop=True)
                return r
            em.op("pe", mmT, reads=["modrow", "onesf"], writes=["ps2"])
            em.op("dve", lambda e: e.tensor_copy(out=self.modT[:], in_=self.ps[2][:, 0:48]),
                  reads=["ps2"], writes=["modT"])
            self.dump("d_modrow", self.modrow[:], [1, 6 * D], F32, ["modrow"])
            self.dump("d_condT", self.condT[:], [128, 8], F32, ["condT"])
            em.op("dve", lambda e: e.tensor_copy(out=self.grow[0:1, 0, :], in_=self.modrow[0:1, 2 * D:3 * D]),
                  reads=["modrow"], writes=["grow"])
            em.op("dve", lambda e: e.tensor_copy(out=self.grow[0:1, 1, :], in_=self.modrow[0:1, 5 * D:6 * D]),
                  reads=["modrow"], writes=["grow"])

    def set_mod(self, l, which):
        nc, em = self.nc, self.em
        nrm = self.norm_mix if which == 0 else self.norm_ffn
        o = 24 * which
        self.load_T(self.nrmT[:], nrm[l:l + 1, :].rearrange("o (k p) -> (o k) p", p=128), 8, "nrmT")
        em.op("dve", lambda e: e.scalar_tensor_tensor(out=self.aT[:], in0=self.modT[:, o + 8:o + 16], scalar=1.0,
                                                     in1=self.nrmT[:], op0=ALU.add, op1=ALU.mult),
              reads=["modT", "nrmT"], writes=["aT"])
        self.bT = self.modT[:, o:o + 8]
        for h in range(2):
            def mm(e, h=h):
                return e.matmul(self.ps[h][:, :], lhsT=self.onesf[0:1, :],
                                rhs=self.grow[0:1, which, h * 512:(h + 1) * 512],
                                start=True, stop=True)
            em.op("pe", mm, reads=["grow", "onesf"], writes=[f"ps{h}"])
            em.op("dve", lambda e, h=h: e.tensor_copy(out=self.gbc[:, h * 512:(h + 1) * 512], in_=self.ps[h][:, :]),
                  reads=[f"ps{h}"], writes=["gbc"])

    def prenorm(self, xt, xkey, outs, pbank, tag):
        em = self.em
        sc = self.pn_sc
        i = self.uid
        self.uid += 1
        b = i % 2
        junk, ss, xn = sc["junk"][0], sc["ss"][b], sc["xn"][b]
        kj, ks, kx = "pnjunk", f"pnss{b}", f"pnxn{b}"
        em.op("dve", lambda e: e.memset(ss[:, 0:1], 0.0), writes=[ks])
        em.op("act", lambda e: e.activation(out=junk[:], in_=xt, func=AF.Square, accum_out=ss[:, 0:1]),
              reads=[xkey, ks], writes=[kj, ks])
        em.op("act", lambda e: e.activation(out=ss[:, 1:2], in_=ss[:, 0:1], func=AF.Sqrt, scale=1.0 / D, bias=self.epsT[:, 0:1]),
              reads=[ks, "epsT"], writes=[ks + "b"])
        em.op("dve", lambda e: e.reciprocal(out=ss[:, 2:3], in_=ss[:, 1:2]), reads=[ks + "b"], writes=[ks + "c"])
        em.op("act", lambda e: e.activation(out=xn[:], in_=xt, func=AF.Identity, scale=ss[:, 2:3]),
              reads=[xkey, ks + "c"], writes=[kx])
        for h in range(2):
            pb = pbank[h]

            def tr(e, h=h, pb=pb):
                for k in range(4):
                    r = e.transpose(out=self.ps[pb][:, k * 128:(k + 1) * 128],
                                    in_=xn[:, (h * 4 + k) * 128:(h * 4 + k + 1) * 128], identity=self.ident[:])
                return r
            em.op("pe", tr, reads=[kx, "ident"], writes=[f"ps{pb}"])
            for (oap, okey) in outs:
                for k in range(4):
                    kk = h * 4 + k
                    eng = "dve" if (kk % 2 == 0) else "act"
                    if eng == "dve":
                        em.op("dve", lambda e, kk=kk, k=k, pb=pb, oap=oap: e.tensor_scalar(
                            out=oap[:, kk, :], in0=self.ps[pb][:, k * 128:(k + 1) * 128],
                            scalar1=self.aT[:, kk:kk + 1], scalar2=self.bT[:, kk:kk + 1],
                            op0=ALU.mult, op1=ALU.add), reads=[f"ps{pb}", "aT", "modT"], writes=[okey])
                    else:
                        em.op("act", lambda e, kk=kk, k=k, pb=pb, oap=oap: e.activation(
                            out=oap[:, kk, :], in_=self.ps[pb][:, k * 128:(k + 1) * 128], func=AF.Identity,
                            scale=self.aT[:, kk:kk + 1], bias=self.bT[:, kk:kk + 1]),
                            reads=[f"ps{pb}", "aT", "modT"], writes=[okey])

    def alloc_pn(self, es):
        self.pn_sc = dict(
            junk=[self.sb(es, "pnjunk", [128, D], BF16)],
            ss=[self.sb(es, f"pnss{b}", [128, 4], F32) for b in range(2)],
            xn=[self.sb(es, f"pnxn{b}", [128, D], F32) for b in range(2)],
        )

    def cast_moe_weights(self, layers):
        em = self.em
        with self.scope() as es:
            st1 = [self.sb(es, f"cst1_{i}", [128, 2048], F32) for i in range(3)]
            ob1 = [self.sb(es, f"cob1_{i}", [128, 2048], BF16) for i in range(3)]
            st2 = [self.sb(es, f"cst2_{i}", [128, 1024], F32) for i in range(3)]
            ob2 = [self.sb(es, f"cob2_{i}", [128, 1024], BF16) for i in range(3)]
            engs = ["dve", "pool", "act"]
            n = 0
            for l in layers:
                for ex in range(min(self.epc, self.opts.get("nex", NE))):
                    w1 = self.moe_w1[l, ex].rearrange("(k p) n -> k p n", p=128)
                    w2 = self.moe_w2[l, ex].rearrange("(k p) n -> k p n", p=128)
                    for k in range(8):
                        i = n % 3
                        n += 1
                        eng = engs[i]
                        em.dma("sp", st1[i][:], w1[k], writes=[f"cst1_{i}"])
                        for half in range(2):
                            src = st1[i][:, half:2048:2]
                            dst = ob1[i][:, half * 1024:(half + 1) * 1024]
                            if eng == "act":
                                em.op(eng, lambda e, s=src, d=dst: e.copy(out=d, in_=s),
                                      reads=[f"cst1_{i}"], writes=[f"cob1_{i}"])
                            else:
                                em.op(eng, lambda e, s=src, d=dst: e.tensor_copy(out=d, in_=s),
                                      reads=[f"cst1_{i}"], writes=[f"cob1_{i}"])
                        c1 = (ex * 8 + k) * 8
                        em.dma("sp", self.w1sh[l][c1:c1 + 8].rearrange("c p n -> p c n"), ob1[i][:].rearrange("p (c n) -> p c n", n=256),
                               reads=[f"cob1_{i}"], writes=[("w1sh", l)])
                        em.dma("sp", st2[i][:], w2[k], writes=[f"cst2_{i}"])
                        if eng == "act":
                            em.op(eng, lambda e, i=i: e.copy(out=ob2[i][:], in_=st2[i][:]),
                                  reads=[f"cst2_{i}"], writes=[f"cob2_{i}"])
                        else:
                            em.op(eng, lambda e, i=i: e.tensor_copy(out=ob2[i][:], in_=st2[i][:]),
                                  reads=[f"cst2_{i}"], writes=[f"cob2_{i}"])
                        c2 = (ex * 8 + k) * 4
                        em.dma("sp", self.w2sh[l][c2:c2 + 4].rearrange("c p n -> p c n"), ob2[i][:].rearrange("p (c n) -> p c n", n=256),
                               reads=[f"cob2_{i}"], writes=[("w2sh", l)])
                if self.nr > 1:
                    for ch in range(self.epc * 64):
                        em.coll_allgather(self.w1sh[l][ch], self.w1g[l][ch], self.nr, reads=[("w1sh", l)], writes=[("w1g", l)])
                    for ch in range(self.epc * 32):
                        em.coll_allgather(self.w2sh[l][ch], self.w2g[l][ch], self.nr, reads=[("w2sh", l)], writes=[("w2g", l)])

    def moe_layer(self, l, src):
        nc, em = self.nc, self.em
        CH = 1024
        NB = CH // 128
        self.set_mod(l, 1)
        with self.scope() as es:
            self.alloc_pn(es)
            xt = [self.sb(es, f"m_xt{i}", [128, D], F32) for i in range(2)]
            hTb = self.sb(es, "m_hTb", [128, 8, CH], BF16)
            hTf = self.sb(es, "m_hTf", [128, 8, 128], F32)
            acc = self.sb(es, "m_acc", [128, NB, D], F32)
            gate = self.sb(es, "m_gate", [128, NB, NE], F32)
            gsc = self.sb(es, "m_gsc", [128, 64], F32)
            gT = self.sb(es, "m_gT", [NE, 128], F32)
            w1s = [self.sb(es, f"m_w1_{i}", [128, 8, 2048], BF16) for i in range(2)]
            w2s = [self.sb(es, f"m_w2_{i}", [128, 8, 1024], BF16) for i in range(2)]
            rw = self.sb(es, "m_rw", [128, 8, NE], F32)
            rbb = self.sb(es, "m_rbb", [128, NE], F32)
            b1T = self.sb(es, "m_b1T", [128, 16, NE], F32)
            b2f = self.sb(es, "m_b2f", [NE, D], F32)
            g1s = [self.sb(es, f"m_g1_{i}", [128, 512], F32) for i in range(2)]
            sgs = [self.sb(es, f"m_sg_{i}", [128, 512], BF16) for i in range(2)]
            l1s = [self.sb(es, f"m_l1_{i}", [128, 512], F32) for i in range(2)]
            uT = self.sb(es, "m_uT", [128, 8, 512], BF16)
            em.dma("sp", rw[:], self.router_w[l].rearrange("(k p) n -> p k n", p=128), writes=["m_rw"])
            em.dma("sp", self.tstage[0:1, 0:NE], self.router_b[l:l + 1, :], writes=["tstage"])
            em.op("pe", lambda e: e.matmul(self.ps[0][:, 0:NE], lhsT=self.onesf[0:1, :], rhs=self.tstage[0:1, 0:NE],
                                           start=True, stop=True), reads=["onesf", "tstage"], writes=["ps0"])
            em.op("dve", lambda e: e.tensor_copy(out=rbb[:], in_=self.ps[0][:, 0:NE]), reads=["ps0"], writes=["m_rbb"])
            em.dma("sp", b2f[:], self.moe_b2[l], writes=["m_b2f"])
            b1rows = acc[0:NE, 0:2, :].rearrange("p a d -> p (a d)")
            em.dma("sp", b1rows, self.moe_b1[l], writes=[("m_acc", 0), ("m_acc", 1)])

            def trb1(e):
                for j in range(16):
                    half, fc = j // 8, j % 8
                    r = e.transpose(out=self.ps[1][:, j * NE:(j + 1) * NE],
                                    in_=b1rows[:, fc * 256 + half:(fc + 1) * 256:2], identity=self.ident[0:NE, 0:NE])
                return r
            em.op("pe", trb1, reads=[("m_acc", 0), ("m_acc", 1), "ident"], writes=["ps1"])
            em.op("dve", lambda e: e.tensor_copy(out=b1T[:].rearrange("p j e -> p (j e)"), in_=self.ps[1][:, 0:512]),
                  reads=["ps1"], writes=["m_b1T"])

            for ch in range(self.opts.get("nch", S // CH)):
                t0 = ch * CH
                xkeys = [("xs", t0 // 512), ("xs", t0 // 512 + 1)]
                for b in range(NB):
                    x_ = xt[b % 2]
                    xk = f"m_xt{b % 2}"
                    em.dma("sp", x_[:], src[t0 + b * 128:t0 + (b + 1) * 128, :], reads=xkeys, writes=[xk])
                    self.prenorm(x_[:], xk,
                                 [(hTb[:, :, b * 128:(b + 1) * 128], "m_hTb"), (hTf[:], "m_hTf")],
                                 (0, 1), "m")

                    def mmr(e):
                        for k in range(8):
                            r = e.matmul(self.ps[2][:, 0:NE], lhsT=hTf[:, k, :], rhs=rw[:, k, :],
                                         start=(k == 0), stop=(k == 7))
                        return r
                    em.op("pe", mmr, reads=["m_hTf", "m_rw"], writes=["ps2"])
                    lg = gsc[:, 0:NE]
                    em.op("dve", lambda e: e.tensor_tensor(out=lg, in0=self.ps[2][:, 0:NE], in1=rbb[:], op=ALU.add),
                          reads=["ps2", "m_rbb"], writes=["m_lg"])
                    mx = gsc[:, 32:40]
                    em.op("dve", lambda e: e.max(out=mx, in_=lg), reads=["m_lg"], writes=["m_mx"])
                    em.op("dve", lambda e: e.tensor_single_scalar(out=gsc[:, 40:41], in_=gsc[:, 32:33], scalar=-1.0, op=ALU.mult), reads=["m_mx"], writes=["m_nmx"])
                    ex_ = gate[:, b, :]
                    em.op("act", lambda e: e.activation(out=ex_, in_=lg, func=AF.Exp, bias=gsc[:, 40:41], scale=1.0),
                          reads=["m_lg", "m_nmx"], writes=["m_gate"])
                    em.op("dve", lambda e: e.scalar_tensor_tensor(out=ex_, in0=lg, scalar=gsc[:, 35:36], in1=ex_,
                                                                 op0=ALU.is_ge, op1=ALU.mult),
                          reads=["m_lg", "m_mx", "m_gate"], writes=["m_gate"])
                    em.op("dve", lambda e: e.tensor_reduce(out=gsc[:, 41:42], in_=ex_, axis=AX.X, op=ALU.add),
                          reads=["m_gate"], writes=["m_gs"])
                    em.op("dve", lambda e: e.reciprocal(out=gsc[:, 42:43], in_=gsc[:, 41:42]), reads=["m_gs"], writes=["m_gr"])
                    em.op("dve", lambda e: e.tensor_single_scalar(out=ex_, in_=ex_, scalar=gsc[:, 42:43], op=ALU.mult), reads=["m_gate", "m_gr"], writes=["m_gate"])
                    em.op("pe", lambda e: e.transpose(out=self.ps[3][0:NE, 0:128], in_=ex_, identity=self.ident[:]),
                          reads=["m_gate", "ident"], writes=["ps3"])
                    em.op("act", lambda e: e.copy(out=gT[:], in_=self.ps[3][0:NE, 0:128]), reads=["ps3"], writes=["m_gT"])
                    for h in range(2):
                        em.op("pe", lambda e: e.matmul(self.ps[4 + h][:, :], lhsT=gT[:], rhs=b2f[:, h * 512:(h + 1) * 512],
                                                       start=True, stop=True), reads=["m_gT", "m_b2f"], writes=[f"ps{4 + h}"])
                        em.op("act", lambda e: e.copy(out=acc[:, b, h * 512:(h + 1) * 512], in_=self.ps[4 + h][:, :]),
                              reads=[f"ps{4 + h}"], writes=[("m_acc", b)])
                if ch == 0:
                    self.dump("d_hTb", hTb[:], [128, 8, CH], BF16, ["m_hTb"])
                    self.dump("d_gate", gate[:], [128, NB, NE], F32, ["m_gate"])
                    self.dump("d_acc0", acc[:], [128, NB, D], F32, [("m_acc", b) for b in range(NB)])
                    self.dump("d_b1T", b1T[:], [128, 16, NE], F32, ["m_b1T"])
                for ex in range(self.opts.get("nex", NE)):
                    wi = ex % 2
                    rk = ("w1g", l) if self.nr > 1 else ("w1sh", l)
                    rk2 = ("w2g", l) if self.nr > 1 else ("w2sh", l)
                    rr, el = ex // self.epc, ex % self.epc
                    for k in range(8):
                        c1 = (el * 8 + k) * 8
                        em.dma("sp", w1s[wi][:, k, :].rearrange("p (c n) -> p c n", n=256),
                               self.w1g[l][c1:c1 + 8, rr * 128:(rr + 1) * 128, :].rearrange("c p n -> p c n"),
                               reads=[rk], writes=[(f"m_w1_{wi}", k)])
                        c2 = (el * 8 + k) * 4
                        em.dma("sp", w2s[wi][:, k, :].rearrange("p (c n) -> p c n", n=256),
                               self.w2g[l][c2:c2 + 4, rr * 128:(rr + 1) * 128, :].rearrange("c p n -> p c n"),
                               reads=[rk2], writes=[(f"m_w2_{wi}", k)])
                    for tl in range(CH // 512):
                        for fc in range(8):
                            i2 = fc % 2
                            pg, pl = 0 + i2, 2 + i2
                            for (pp, off) in ((pg, 0), (pl, 1024)):
                                def mm1(e):
                                    for k in range(8):
                                        r = e.matmul(self.ps[pp][:, :], lhsT=w1s[wi][:, k, off + fc * 128:off + (fc + 1) * 128],
                                                     rhs=hTb[:, k, tl * 512:(tl + 1) * 512], start=(k == 0), stop=(k == 7))
                                    return r
                                em.op("pe", mm1, reads=[(f"m_w1_{wi}", k) for k in range(8)] + ["m_hTb"], writes=[f"ps{pp}"])
                            g1, sg, l1 = g1s[i2], sgs[i2], l1s[i2]
                            em.op("dve", lambda e: e.tensor_scalar(
                                out=g1[:], in0=self.ps[pg][:, :], scalar1=b1T[:, fc, ex:ex + 1], scalar2=7.0,
                                op0=ALU.add, op1=ALU.min), reads=[f"ps{pg}", "m_b1T"], writes=[f"m_g1_{i2}"])
                            em.op("act", lambda e: e.activation(out=sg[:], in_=g1[:], func=AF.Sigmoid, scale=1.702),
                                  reads=[f"m_g1_{i2}"], writes=[f"m_sg_{i2}"])
                            em.op("dve", lambda e: e.tensor_scalar(
                                out=l1[:], in0=self.ps[pl][:, :], scalar1=b1T[:, 8 + fc, ex:ex + 1], scalar2=7.0,
                                op0=ALU.add, op1=ALU.min), reads=[f"ps{pl}", "m_b1T"], writes=[f"m_l1_{i2}"])
                            em.op("dve", lambda e: e.tensor_scalar(
                                out=l1[:], in0=l1[:], scalar1=-7.0, scalar2=1.0, op0=ALU.max, op1=ALU.add),
                                reads=[f"m_l1_{i2}"], writes=[f"m_l1_{i2}"])
                            em.op("pool", lambda e: e.tensor_tensor(out=g1[:], in0=g1[:], in1=sg[:], op=ALU.mult),
                                  reads=[f"m_g1_{i2}", f"m_sg_{i2}"], writes=[f"m_g1_{i2}"])
                            em.op("pool", lambda e: e.tensor_tensor(out=uT[:, fc, :], in0=g1[:], in1=l1[:], op=ALU.mult),
                                  reads=[f"m_g1_{i2}", f"m_l1_{i2}"], writes=["m_uT"])
                        for tb in range(4):
                            b = tl * 4 + tb
                            for h in range(2):
                                po = 4 + (tb * 2 + h) % 4

                                def mmo(e):
                                    for k in range(8):
                                        r = e.matmul(self.ps[po][:, :], lhsT=uT[:, k, tb * 128:(tb + 1) * 128],
                                                     rhs=w2s[wi][:, k, h * 512:(h + 1) * 512], start=(k == 0), stop=(k == 7))
                                    return r
                                em.op("pe", mmo, reads=["m_uT"] + [(f"m_w2_{wi}", k) for k in range(8)], writes=[f"ps{po}"])
                                a = acc[:, b, h * 512:(h + 1) * 512]
                                em.op("dve", lambda e: e.scalar_tensor_tensor(
                                    out=a, in0=self.ps[po][:, :], scalar=gate[:, b, ex:ex + 1], in1=a,
                                    op0=ALU.mult, op1=ALU.add), reads=[f"ps{po}", "m_gate", ("m_acc", b)], writes=[("m_acc", b)])
                if ch == 0:
                    self.dump("d_acc1", acc[:], [128, NB, D], F32, [("m_acc", b) for b in range(NB)])
                    self.dump("d_gbc", self.gbc[:], [128, D], F32, ["gbc"])
                    self.dump("d_aT", self.aT[:], [128, 8], F32, ["aT"])
                    self.dump("d_modT", self.modT[:], [128, 48], F32, ["modT"])
                for b in range(NB):
                    x_ = xt[b % 2]
                    xk = f"m_xt{b % 2}"
                    em.dma("sp", x_[:], src[t0 + b * 128:t0 + (b + 1) * 128, :], reads=xkeys, writes=[xk])
                    em.op("pool", lambda e: e.tensor_tensor(out=acc[:, b, :], in0=acc[:, b, :], in1=self.gbc[:], op=ALU.mult),
                          reads=[("m_acc", b), "gbc"], writes=[("m_acc", b)])
                    em.op("pool", lambda e: e.tensor_tensor(out=acc[:, b, :], in0=acc[:, b, :], in1=x_[:], op=ALU.add),
                          reads=[("m_acc", b), xk], writes=[("m_acc", b)])
                em.dma("sp", self.y[t0:t0 + CH, :].rearrange("(b p) d -> p b d", p=128), acc[:],
                       reads=[("m_acc", b) for b in range(NB)], writes=xkeys)

    def conv_layer(self, l, src):
        nc, em = self.nc, self.em
        j = l // 2
        TL = 512
        self.set_mod(l, 0)
        with self.scope() as es:
            self.alloc_pn(es)
            pw1 = self.sb(es, "c_pw1", [128, 8, 2048], BF16)
            pw2 = self.sb(es, "c_pw2", [128, 8, 1024], BF16)
            stg = [self.sb(es, f"c_stg{i}", [128, 2048], F32) for i in range(2)]
            b1T = self.sb(es, "c_b1T", [128, 16], F32)
            dwT = self.sb(es, "c_dwT", [128, 8, 31], F32)
            dwbT = self.sb(es, "c_dwbT", [128, 8], F32)
            lngT = self.sb(es, "c_lngT", [128, 8], F32)
            lnbT = self.sb(es, "c_lnbT", [128, 8], F32)
            brow = self.sb(es, "c_brow", [1, D], F32)
            bbc = self.sb(es, "c_bbc", [128, D], F32)
            xt = [self.sb(es, f"c_xt{i}", [128, 4, D], F32) for i in range(2)]
            hT = self.sb(es, "c_hT", [128, 8, TL], BF16)
            ub = self.sb(es, "c_ub", [128, 8, 30 + TL], F32)
            sig = [self.sb(es, f"c_sig{i}", [128, TL], F32) for i in range(2)]
            v = self.sb(es, "c_v", [128, 8, TL], F32)
            vsq = [self.sb(es, f"c_vsq{i}", [128, TL], F32) for i in range(2)]
            st = self.sb(es, "c_st", [128, 4, TL], F32)
            zT = self.sb(es, "c_zT", [128, 8, TL], BF16)
            w1v = self.conv_pw1_w[j].rearrange("(k p) n -> k p n", p=128)
            w2v = self.conv_pw2_w[j].rearrange("(k p) n -> k p n", p=128)
            for k in range(8):
                em.dma("sp", stg[k % 2][:], w1v[k], writes=[f"c_stg{k % 2}"])
                em.op("dve" if k % 2 == 0 else "pool", lambda e, k=k: e.tensor_copy(out=pw1[:, k, :], in_=stg[k % 2][:]),
                      reads=[f"c_stg{k % 2}"], writes=["c_pw1"])
            for k in range(8):
                em.dma("sp", stg[k % 2][:, 0:1024], w2v[k], writes=[f"c_stg{k % 2}"])
                em.op("dve" if k % 2 == 0 else "pool", lambda e, k=k: e.tensor_tensor(
                    out=pw2[:, k, :], in0=stg[k % 2][:, 0:1024], in1=self.gbc[:], op=ALU.mult),
                    reads=[f"c_stg{k % 2}", "gbc"], writes=["c_pw2"])
            self.load_T(b1T[:], self.conv_pw1_b[j:j + 1, :].rearrange("o (k p) -> (o k) p", p=128), 16, "c_b1T")
            for k in range(8):
                self.load_T(dwT[:, k, :], self.conv_dw_w[j][:, k * 128:(k + 1) * 128], 31, "c_dwT")
            self.load_T(dwbT[:], self.conv_dw_b[j:j + 1, :].rearrange("o (k p) -> (o k) p", p=128), 8, "c_dwbT")
            self.load_T(lngT[:], self.conv_ln_g[j:j + 1, :].rearrange("o (k p) -> (o k) p", p=128), 8, "c_lngT")
            self.load_T(lnbT[:], self.conv_ln_b[j:j + 1, :].rearrange("o (k p) -> (o k) p", p=128), 8, "c_lnbT")
            em.dma("sp", brow[:], self.conv_pw2_b[j:j + 1, :], writes=["c_brow"])
            for h in range(2):
                em.op("pe", lambda e, h=h: e.matmul(self.ps[h][:, :], lhsT=self.onesf[0:1, :],
                                                   rhs=brow[0:1, h * 512:(h + 1) * 512], start=True, stop=True),
                      reads=["onesf", "c_brow"], writes=[f"ps{h}"])
                em.op("dve", lambda e, h=h: e.tensor_tensor(out=bbc[:, h * 512:(h + 1) * 512], in0=self.ps[h][:, :],
                                                           in1=self.gbc[:, h * 512:(h + 1) * 512], op=ALU.mult),
                      reads=[f"ps{h}", "gbc"], writes=["c_bbc"])
            em.op("pool", lambda e: e.memset(ub[:, :, 0:30], 0.0), writes=["c_ub"])

            for ti in range(S // TL):
                t0 = ti * TL
                x_ = xt[ti % 2]
                xk = f"c_xt{ti % 2}"
                em.dma("sp", x_[:], src[t0:t0 + TL, :].rearrange("(b p) d -> p b d", p=128),
                       reads=[("xs", ti)], writes=[xk])
                for b in range(4):
                    self.prenorm(x_[:, b, :], xk, [(hT[:, :, b * 128:(b + 1) * 128], "c_hT")], (0, 1), "c")
                for c in range(8):
                    i2 = c % 2
                    pa, pb = 2 + i2, 4 + i2
                    for (pp, n) in ((pb, 8 + c), (pa, c)):
                        def mm(e, pp=pp, n=n):
                            for k in range(8):
                                r = e.matmul(self.ps[pp][:, :], lhsT=pw1[:, k, n * 128:(n + 1) * 128], rhs=hT[:, k, :],
                                             start=(k == 0), stop=(k == 7))
                            return r
                        em.op("pe", mm, reads=["c_pw1", "c_hT"], writes=[f"ps{pp}"])
                    em.op("act", lambda e, c=c, pb=pb, i2=i2: e.activation(out=sig[i2][:], in_=self.ps[pb][:, :], func=AF.Sigmoid,
                                                                         bias=b1T[:, 8 + c:9 + c], scale=1.0),
                          reads=[f"ps{pb}", "c_b1T"], writes=[f"c_sig{i2}"])
                    em.op("dve", lambda e, c=c, pa=pa, i2=i2: e.scalar_tensor_tensor(
                        out=ub[:, c, 30:30 + TL], in0=self.ps[pa][:, :], scalar=b1T[:, c:c + 1], in1=sig[i2][:],
                        op0=ALU.add, op1=ALU.mult), reads=[f"ps{pa}", "c_b1T", f"c_sig{i2}"], writes=[("c_ubm", c)])
                for tap in range(31):
                    for c in range(8):
                        eng = "dve"
                        if tap == 0:
                            em.op(eng, lambda e, c=c: e.tensor_scalar(
                                out=v[:, c, :], in0=ub[:, c, 0:TL], scalar1=dwT[:, c, 0:1], scalar2=dwbT[:, c:c + 1],
                                op0=ALU.mult, op1=ALU.add), reads=[("c_ubm", c), "c_ub", "c_dwT", "c_dwbT"], writes=[("c_v", c)])
                        else:
                            em.op(eng, lambda e, c=c, tap=tap: e.scalar_tensor_tensor(
                                out=v[:, c, :], in0=ub[:, c, tap:tap + TL], scalar=dwT[:, c, tap:tap + 1], in1=v[:, c, :],
                                op0=ALU.mult, op1=ALU.add), reads=[("c_ubm", c), "c_ub", ("c_v", c)], writes=[("c_v", c)])
                em.op("pool", lambda e: e.tensor_copy(out=ub[:, :, 0:30], in_=ub[:, :, TL:TL + 30]),
                      reads=[("c_ubm", c) for c in range(8)] + ["c_ub"], writes=["c_ub"])
                for c in range(8):
                    em.op("act", lambda e, c=c: e.activation(out=vsq[c % 2][:], in_=v[:, c, :], func=AF.Square),
                          reads=[("c_v", c)], writes=[f"c_vsq{c % 2}"])
                    em.op("pe", lambda e, c=c: e.matmul(self.ps[6][:, :], lhsT=self.onesf[:], rhs=v[:, c, :],
                                                       start=(c == 0), stop=(c == 7)),
                          reads=[("c_v", c), "onesf"], writes=["ps6"])
                    em.op("pe", lambda e, c=c: e.matmul(self.ps[7][:, :], lhsT=self.onesf[:], rhs=vsq[c % 2][:],
                                                       start=(c == 0), stop=(c == 7)),
                          reads=[f"c_vsq{c % 2}", "onesf"], writes=["ps7"])
                mean, msq, var, rstd = st[:, 0, :], st[:, 1, :], st[:, 2, :], st[:, 3, :]
                em.op("act", lambda e: e.activation(out=mean, in_=self.ps[6][:, :], func=AF.Copy, scale=1.0 / D),
                      reads=["ps6"], writes=["c_mean"])
                em.op("dve", lambda e: e.tensor_tensor(out=msq, in0=mean, in1=mean, op=ALU.mult),
                      reads=["c_mean"], writes=["c_msq"])
                em.op("dve", lambda e: e.scalar_tensor_tensor(out=var, in0=self.ps[7][:, :], scalar=1.0 / D, in1=msq,
                                                             op0=ALU.mult, op1=ALU.subtract),
                      reads=["ps7", "c_msq"], writes=["c_var"])
                em.op("act", lambda e: e.activation(out=rstd, in_=var, func=AF.Sqrt, scale=1.0, bias=self.epsT[:, 0:1]),
                      reads=["c_var", "epsT"], writes=["c_rstd"])
                em.op("dve", lambda e: e.reciprocal(out=rstd, in_=rstd), reads=["c_rstd"], writes=["c_rstd"])
                for c in range(8):
                    eng = "dve" if c % 2 == 0 else "pool"
                    em.op(eng, lambda e, c=c: e.tensor_tensor(out=v[:, c, :], in0=v[:, c, :], in1=mean, op=ALU.subtract),
                          reads=[("c_v", c), "c_mean"], writes=[("c_v", c)])
                    em.op(eng, lambda e, c=c: e.tensor_tensor(out=v[:, c, :], in0=v[:, c, :], in1=rstd, op=ALU.mult),
                          reads=[("c_v", c), "c_rstd"], writes=[("c_v", c)])
                    em.op("act", lambda e, c=c: e.activation(out=zT[:, c, :], in_=v[:, c, :], func=AF.Silu,
                                                             scale=lngT[:, c:c + 1], bias=lnbT[:, c:c + 1]),
                          reads=[("c_v", c), "c_lngT", "c_lnbT"], writes=["c_zT"])
                for b in range(4):
                    for h in range(2):
                        po = 2 + (b * 2 + h) % 4

                        def mmo(e, b=b, h=h, po=po):
                            for k in range(8):
                                r = e.matmul(self.ps[po][:, :], lhsT=zT[:, k, b * 128:(b + 1) * 128],
                                             rhs=pw2[:, k, h * 512:(h + 1) * 512], start=(k == 0), stop=(k == 7))
                            return r
                        em.op("pe", mmo, reads=["c_zT", "c_pw2"], writes=[f"ps{po}"])
                        xa = x_[:, b, h * 512:(h + 1) * 512]
                        em.op("dve", lambda e, xa=xa, po=po: e.tensor_tensor(out=xa, in0=self.ps[po][:, :], in1=xa, op=ALU.add),
                              reads=[f"ps{po}", xk], writes=[xk])
                        em.op("pool", lambda e, xa=xa, h=h: e.tensor_tensor(out=xa, in0=xa, in1=bbc[:, h * 512:(h + 1) * 512], op=ALU.add),
                              reads=[xk, "c_bbc"], writes=[xk])
                em.dma("sp", self.y[t0:t0 + TL, :].rearrange("(b p) d -> p b d", p=128), x_[:],
                       reads=[xk], writes=[("xs", ti)])

    def bcast_row(self, dst, row_ap, n, key, rkey):
        em = self.em
        em.op("pe", lambda e: e.matmul(self.ps[7][:, 0:n], lhsT=self.onesf[0:1, :], rhs=row_ap, start=True, stop=True),
              reads=["onesf", rkey], writes=["ps7"])
        em.op("dve", lambda e: e.tensor_copy(out=dst, in_=self.ps[7][:, 0:n]), reads=["ps7"], writes=[key])

    def grp_norm(self, psb, w, G, gain_bc, out, okey, sc):
        em = self.em
        sq, st = sc
        pk = f"ps{psb}"
        em.op("act", lambda e: e.activation(out=sq[:, 0:w], in_=self.ps[psb][:, 0:w], func=AF.Square), reads=[pk], writes=["gn_sq"])
        em.op("dve", lambda e: e.tensor_reduce(out=st[:, 0:G], in_=sq[:, 0:w].rearrange("p (g d) -> p g d", d=64),
                                               axis=AX.X, op=ALU.add), reads=["gn_sq"], writes=["gn_st"])
        em.op("act", lambda e: e.activation(out=st[:, 8:8 + G], in_=st[:, 0:G], func=AF.Sqrt, scale=1.0 / 64, bias=self.epsT[:, 0:1]),
              reads=["gn_st", "epsT"], writes=["gn_st2"])
        em.op("dve", lambda e: e.reciprocal(out=st[:, 16:16 + G], in_=st[:, 8:8 + G]), reads=["gn_st2"], writes=["gn_st3"])
        for g in range(G):
            if g % 2 == 1:
                em.op("act", lambda e, g=g: e.activation(out=out[:, g * 64:(g + 1) * 64], in_=self.ps[psb][:, g * 64:(g + 1) * 64],
                                                         func=AF.Identity, scale=st[:, 16 + g:17 + g]),
                      reads=[pk, "gn_st3"], writes=[okey])
            else:
                em.op("dve", lambda e, g=g: e.tensor_single_scalar(out=out[:, g * 64:(g + 1) * 64], in_=self.ps[psb][:, g * 64:(g + 1) * 64],
                                                                   scalar=st[:, 16 + g:17 + g], op=ALU.mult),
                      reads=[pk, "gn_st3"], writes=[okey])
        em.op("pool", lambda e: e.tensor_tensor(out=out[:, 0:w], in0=out[:, 0:w], in1=gain_bc[:, 0:w], op=ALU.mult),
              reads=[okey, "gains"], writes=[okey])

    def attn_layer(self, l, src):
        nc, em = self.nc, self.em
        j = l // 2
        lam_init = 0.8 - 0.6 * math.exp(-0.3 * l)
        self.set_mod(l, 0)
        sc = self.scratch
        dqT = sc(f"dqT{j}", [4, 128, S], BF16); dkT = sc(f"dkT{j}", [4, 128, S], BF16)
        dvd = sc(f"dvd{j}", [4, S, 128], BF16)
        nqT = sc(f"nqT{j}", [4, 128, S], BF16)
        ksT = sc(f"ksT{j}", [2, 128, S], BF16); kwT = sc(f"kwT{j}", [2, 128, S], BF16)
        vsd = sc(f"vsd{j}", [2, S, 64], BF16); vwd = sc(f"vwd{j}", [2, S, 64], BF16)
        kcTr = sc(f"kcTr{j}", [128, S], BF16); vcTr = sc(f"vcTr{j}", [128, S], BF16)
        gat = sc(f"gat{j}", [S, 24], F32)
        att = sc(f"att{j}", [S, D], F32)
        ph = self.opts.get("aphases", ("proj", "diff", "nsa", "out"))
        if "proj" in ph:
            self.attn_proj(l, j, src, dqT, dkT, dvd, nqT, ksT, kwT, vsd, vwd, kcTr, vcTr, gat)
        if "nsa" in ph:
            self.attn_nsa(l, j, nqT, ksT, kwT, vsd, vwd, kcTr, vcTr, gat, att)
        if "diff" in ph:
            self.attn_diff(l, j, lam_init, dqT, dkT, dvd, att)
        if "out" in ph:
            self.attn_out(l, j, src, att)

    def attn_proj(self, l, j, src, dqT, dkT, dvd, nqT, ksT, kwT, vsd, vwd, kcTr, vcTr, gat):
        nc, em = self.nc, self.em
        with self.scope() as es:
            self.alloc_pn(es)
            win = self.sb(es, "a_win", [128, 8, W_IN], BF16)
            stg = [self.sb(es, f"a_stg{i}", [128, W_IN], F32) for i in range(2)]
            gains = self.sb(es, "a_gains", [128, 5, 512], F32)
            grow = self.sb(es, "a_grow", [1, 64], F32)
            xt = [self.sb(es, f"a_xt{i}", [128, D], F32) for i in range(2)]
            hT = self.sb(es, "a_hT", [128, 8, 128], BF16)
            sq = self.sb(es, "a_sq", [128, 512], F32)
            st = self.sb(es, "a_st", [128, 24], F32)
            qn = self.sb(es, "a_qn", [128, 512], F32)
            kdup = self.sb(es, "a_kdup", [128, 2, 128], F32)
            ktmp = self.sb(es, "a_ktmp", [128, 128], F32)
            ksq = self.sb(es, "a_ksq", [128, 128], F32)
            sq4 = self.sb(es, "a_sq4", [128, 4, 128], BF16)
            sk4 = self.sb(es, "a_sk4", [128, 4, 128], BF16)
            sn4 = self.sb(es, "a_sn4", [128, 4, 128], BF16)
            sv = self.sb(es, "a_sv", [128, 512], BF16)
            scv = self.sb(es, "a_scv", [128, 2, 128], BF16)
            sks = self.sb(es, "a_sks", [128, 2, 128], BF16)
            skw = self.sb(es, "a_skw", [128, 2, 128], BF16)
            svs = self.sb(es, "a_svs", [128, 128], BF16)
            svw = self.sb(es, "a_svw", [128, 128], BF16)
            sg = self.sb(es, "a_sg", [128, 24], F32)
            wv = self.attn_w_in[j].rearrange("(k p) n -> k p n", p=128)
            for k in range(8):
                em.dma("sp", stg[k % 2][:], wv[k], writes=[f"a_stg{k % 2}"])
                em.op("dve" if k % 2 == 0 else "pool", lambda e, k=k: e.tensor_copy(out=win[:, k, :], in_=stg[k % 2][:]),
                      reads=[f"a_stg{k % 2}"], writes=["a_win"])
            gsrc = [self.diff_qk_gain[j, 0:1, :], self.diff_qk_gain[j, 1:2, :], self.nsa_q_gain[j:j + 1, :],
                    self.nsa_k_gain[j, 1:2, :], self.nsa_k_gain[j, 2:3, :]]
            for gi, ga in enumerate(gsrc):
                em.dma("sp", grow[:], ga, writes=["a_grow"])
                self.bcast_row(gains[:, gi, 0:64], grow[0:1, :], 64, "gains", "a_grow")
                for r in range(1, 8):
                    em.op("dve", lambda e, gi=gi, r=r: e.tensor_copy(out=gains[:, gi, r * 64:(r + 1) * 64], in_=gains[:, gi, 0:64]),
                          reads=["gains"], writes=["gains"])
            for tb in range(self.opts.get("ntb", NT)):
                t0 = tb * 128
                x_ = xt[tb % 2]
                xk = f"a_xt{tb % 2}"
                em.dma("sp", x_[:], src[t0:t0 + 128, :], reads=[("xs", t0 // 512)], writes=[xk])
                self.prenorm(x_[:], xk, [(hT[:], "a_hT")], (0, 1), "a")
                for nb in range(6):
                    n0 = nb * 512
                    n1 = min(W_IN, n0 + 512)

                    def mm(e, nb=nb, n0=n0, n1=n1):
                        for k in range(8):
                            r = e.matmul(self.ps[2 + nb][:, 0:n1 - n0], lhsT=hT[:, k, :], rhs=win[:, k, n0:n1],
                                         start=(k == 0), stop=(k == 7))
                        return r
                    em.op("pe", mm, reads=["a_hT", "a_win"], writes=[f"ps{2 + nb}"])

                def tr4(inp, ikey, n, pb, dst, dkey):
                    def f(e):
                        for i in range(n):
                            r = e.transpose(out=self.ps[pb][:, i * 128:(i + 1) * 128], in_=inp[:, i * 128:(i + 1) * 128],
                                            identity=self.ident[:])
                        return r
                    em.op("pe", f, reads=[ikey, "ident"], writes=[f"ps{pb}"])
                    em.op("act", lambda e: e.copy(out=dst, in_=self.ps[pb][:, 0:n * 128]), reads=[f"ps{pb}"], writes=[dkey])
                self.grp_norm(2, 512, 8, gains[:, 0, :], qn, "a_qn", (sq, st))
                tr4(qn, "a_qn", 4, 0, sq4[:].rearrange("p h t -> p (h t)"), "a_sq4")
                em.dma("sp", dqT[:, :, t0:t0 + 128].rearrange("h p t -> p h t"), sq4[:], reads=["a_sq4"], writes=[("dqT", tb)])
                self.grp_norm(3, 512, 8, gains[:, 1, :], qn, "a_qn", (sq, st))
                tr4(qn, "a_qn", 4, 1, sk4[:].rearrange("p h t -> p (h t)"), "a_sk4")
                em.dma("sp", dkT[:, :, t0:t0 + 128].rearrange("h p t -> p h t"), sk4[:], reads=["a_sk4"], writes=[("dkT", tb)])
                em.op("act", lambda e: e.copy(out=sv[:], in_=self.ps[4][:, :]), reads=["ps4"], writes=["a_sv"])
                em.dma("sp", dvd[:, t0:t0 + 128, :].rearrange("h t e -> t h e"), sv[:].rearrange("t (h e) -> t h e", h=4),
                       reads=["a_sv"], writes=[("dvd", tb)])
                self.grp_norm(5, 512, 8, gains[:, 2, :], qn, "a_qn", (sq, st))
                tr4(qn, "a_qn", 4, 0, sn4[:].rearrange("p h t -> p (h t)"), "a_sn4")
                em.dma("sp", nqT[:, :, t0:t0 + 128].rearrange("h p t -> p h t"), sn4[:], reads=["a_sn4"], writes=[("nqT", tb)])
                em.op("dve", lambda e: e.tensor_copy(out=qn[:, 0:256], in_=self.ps[6][:, 0:256]), reads=["ps6"], writes=["a_qn"])
                tr4(qn, "a_qn", 2, 1, scv[:].rearrange("p h t -> p (h t)"), "a_scv")
                em.dma("sp", kcTr[:, t0:t0 + 128], scv[:, 0, :], reads=["a_scv"], writes=[("kcTr", tb)])
                em.dma("sp", vcTr[:, t0:t0 + 128], scv[:, 1, :], reads=["a_scv"], writes=[("vcTr", tb)])
                for (psb, c0, gi, stg_, skey, dst) in ((6, 256, 3, sks, "a_sks", ksT), (7, 0, 4, skw, "a_skw", kwT)):
                    em.op("dve", lambda e, psb=psb, c0=c0: e.tensor_copy(out=ktmp[:], in_=self.ps[psb][:, c0:c0 + 128]),
                          reads=[f"ps{psb}"], writes=["a_ktmp"])
                    em.op("act", lambda e, psb=psb, c0=c0: e.activation(out=ksq[:], in_=self.ps[psb][:, c0:c0 + 128], func=AF.Square),
                          reads=[f"ps{psb}"], writes=["a_ksq"])
                    em.op("dve", lambda e: e.tensor_reduce(out=st[:, 0:2], in_=ksq[:].rearrange("p (g d) -> p g d", d=64),
                                                           axis=AX.X, op=ALU.add), reads=["a_ksq"], writes=["gn_st"])
                    em.op("act", lambda e: e.activation(out=st[:, 8:10], in_=st[:, 0:2], func=AF.Sqrt, scale=1.0 / 64, bias=self.epsT[:, 0:1]),
                          reads=["gn_st", "epsT"], writes=["gn_st2"])
                    em.op("dve", lambda e: e.reciprocal(out=st[:, 16:18], in_=st[:, 8:10]), reads=["gn_st2"], writes=["gn_st3"])
                    for h in range(2):
                        for r in range(2):
                            em.op("dve", lambda e, h=h, r=r, gi=gi: e.scalar_tensor_tensor(
                                out=kdup[:, h, r * 64:(r + 1) * 64], in0=ktmp[:, h * 64:(h + 1) * 64], scalar=st[:, 16 + h:17 + h],
                                in1=gains[:, gi, 0:64], op0=ALU.mult, op1=ALU.mult),
                                reads=["a_ktmp", "gn_st3", "gains"], writes=["a_kdup"])
                    tr4(kdup[:].rearrange("p h t -> p (h t)"), "a_kdup", 2, 0, stg_[:].rearrange("p h t -> p (h t)"), skey)
                    em.dma("sp", dst[:, :, t0:t0 + 128].rearrange("h p t -> p h t"), stg_[:], reads=[skey], writes=[(skey, tb)])
                em.op("act", lambda e: e.copy(out=svs[:], in_=self.ps[6][:, 384:512]), reads=["ps6"], writes=["a_svs"])
                em.dma("sp", vsd[:, t0:t0 + 128, :].rearrange("h t d -> t h d"), svs[:].rearrange("t (h d) -> t h d", h=2),
                       reads=["a_svs"], writes=[("vsd", tb)])
                em.op("act", lambda e: e.copy(out=svw[:], in_=self.ps[7][:, 128:256]), reads=["ps7"], writes=["a_svw"])
                em.dma("sp", vwd[:, t0:t0 + 128, :].rearrange("h t d -> t h d"), svw[:].rearrange("t (h d) -> t h d", h=2),
                       reads=["a_svw"], writes=[("vwd", tb)])
                em.op("act", lambda e: e.activation(out=sg[:], in_=self.ps[7][:, 256:280], func=AF.Sigmoid), reads=["ps7"], writes=["a_sg"])
                em.dma("sp", gat[t0:t0 + 128, :], sg[:], reads=["a_sg"], writes=[("gat", tb)])

    def att_tiles(self, q_ap, qkey, items, dvp, obanks, per_bank, P, tag):
        em = self.em
        for b in obanks:
            em.op("dve", lambda e, b=b: e.memset(self.ps[b][:, :], 0.0), writes=[f"ps{b}"])
        for n, it in enumerate(items):
            sbk = n % 2
            qlo, qhi = it["qlo"], it["qhi"]

            def mms(e, it=it, sbk=sbk, qlo=qlo, qhi=qhi):
                nm = len(it["masks"])
                r = e.matmul(self.ps[sbk][:, qlo:qhi], lhsT=it["kT"], rhs=q_ap[:, qlo:qhi], start=True, stop=(nm == 0))
                for mi, (ml, mr, c0, c1, _) in enumerate(it["masks"]):
                    r = e.matmul(self.ps[sbk][:, c0:c1], lhsT=ml, rhs=mr, start=False, stop=(mi == nm - 1))
                return r
            mkeys = [k for m in it["masks"] for k in m[4]]
            em.op("pe", mms, reads=[qkey, it["kkey"]] + mkeys, writes=[f"ps{sbk}"])
            pt = P[sbk]
            em.op("act", lambda e, pt=pt, sbk=sbk, qlo=qlo, qhi=qhi: e.activation(
                out=pt[:, qlo:qhi], in_=self.ps[sbk][:, qlo:qhi], func=AF.Exp, scale=0.125),
                reads=[f"ps{sbk}"], writes=[f"{tag}P{sbk}"])

            def mmo(e, it=it, pt=pt, qlo=qlo, qhi=qhi):
                for qb in range(qlo // 128, qhi // 128):
                    bk = obanks[qb // per_bank]
                    c = (qb % per_bank) * dvp
                    r = e.matmul(self.ps[bk][:, c:c + dvp], lhsT=pt[:, qb * 128:(qb + 1) * 128], rhs=it["v"],
                                 start=False, stop=False, skip_group_check=True)
                return r
            em.op("pe", mmo, reads=[f"{tag}P{sbk}", it["vkey"]], writes=[f"ps{b}" for b in obanks])

    def attn_diff(self, l, j, lam_init, dqT, dkT, dvd, att):
        nc, em = self.nc, self.em
        with self.scope() as es:
            qT = self.sb(es, "d_qT", [128, S], BF16)
            kT = self.sb(es, "d_kT", [128, S], BF16)
            vv = self.sb(es, "d_v", [128, NT, 129], BF16)
            P = [self.sb(es, f"d_P{i}", [128, 512], BF16) for i in range(2)]
            tri = self.sb(es, "d_tri", [128, 128], BF16)
            lrow = self.sb(es, "d_lrow", [1, 264], F32)
            lam = self.sb(es, "d_lam", [128, 2], F32)
            subg = self.sb(es, "d_subg", [128, 128], F32)
            o1 = self.sb(es, "d_o1", [128, 128], F32)
            o2 = self.sb(es, "d_o2", [128, 128], F32)
            rs = self.sb(es, "d_rs", [128, 8], F32)
            oo = [self.sb(es, f"d_oo{i}", [128, 4, 128], F32) for i in range(2)]
            em.dma("sp", tri[:], self.din["trimask"][:, :], writes=["d_tri"])
            em.dma("sp", lrow[0:1, 0:256], self.diff_lambda[j:j + 1].rearrange("o a d -> o (a d)"), writes=["d_lrow"])
            for i in range(2):
                em.op("dve", lambda e, i=i: e.tensor_tensor(out=lrow[0:1, i * 128:i * 128 + 64], in0=lrow[0:1, i * 128:i * 128 + 64],
                                                         in1=lrow[0:1, i * 128 + 64:i * 128 + 128], op=ALU.mult),
                      reads=["d_lrow"], writes=["d_lrow"])
                em.op("dve", lambda e, i=i: e.tensor_reduce(out=lrow[0:1, 256 + i:257 + i], in_=lrow[0:1, i * 128:i * 128 + 64],
                                                         axis=AX.X, op=ALU.add), reads=["d_lrow"], writes=["d_lrow"])
            em.op("act", lambda e: e.activation(out=lrow[0:1, 258:260], in_=lrow[0:1, 256:258], func=AF.Exp), reads=["d_lrow"], writes=["d_lrow"])
            em.op("dve", lambda e: e.tensor_tensor(out=lrow[0:1, 260:261], in0=lrow[0:1, 258:259], in1=lrow[0:1, 259:260], op=ALU.subtract),
                  reads=["d_lrow"], writes=["d_lrow"])
            em.op("dve", lambda e: e.tensor_scalar(out=lrow[0:1, 261:262], in0=lrow[0:1, 260:261], scalar1=lam_init, scalar2=-1.0,
                                                   op0=ALU.add, op1=ALU.mult), reads=["d_lrow"], writes=["d_lrow"])
            self.bcast_row(lam[:, 0:1], lrow[0:1, 261:262], 1, "d_lam", "d_lrow")
            em.dma("sp", lrow[0:1, 0:128], self.diff_subln[j:j + 1, :], writes=["d_lrow"])
            self.bcast_row(subg[:], lrow[0:1, 0:128], 128, "d_subg", "d_lrow")
            em.op("dve", lambda e: e.tensor_single_scalar(out=subg[:], in_=subg[:], scalar=1.0 - lam_init, op=ALU.mult),
                  reads=["d_subg"], writes=["d_subg"])
            em.op("dve", lambda e: e.memset(vv[:, :, 128:129], 1.0), writes=["d_vones"])
            nqt = self.opts.get("nqt", S // 512)
            for h in range(4):
                em.dma("sp", qT[:], dqT[h], reads=[("dqT", tb) for tb in range(NT)], writes=["d_qT"])
                em.dma("sp", kT[:], dkT[h], reads=[("dkT", tb) for tb in range(NT)], writes=["d_kT"])
                em.dma("sp", vv[:, :, 0:128], dvd[h].rearrange("(n p) e -> p n e", p=128),
                       reads=[("dvd", tb) for tb in range(NT)], writes=["d_v"])
                for qi in range(nqt):
                    for m in range(2):
                        items = []
                        for kj in range(4 * qi + 4):
                            r = kj - 4 * qi
                            it = dict(kT=kT[64 * m:64 * m + 64, kj * 128:(kj + 1) * 128], kkey="d_kT", v=vv[:, kj, :], vkey="d_v",
                                      qlo=0, qhi=512, masks=[])
                            if r >= 0:
                                it["qlo"] = r * 128
                                it["masks"] = [(self.identb[:], tri[:], r * 128, (r + 1) * 128, ["identb", "d_tri"])]
                            items.append(it)
                        self.att_tiles(qT[64 * m:64 * m + 64, qi * 512:(qi + 1) * 512], "d_qT", items, 129,
                                       (2 + 2 * m, 3 + 2 * m), 2, P, "d_")
                    o_ = oo[qi % 2]
                    ok = f"d_oo{qi % 2}"
                    for qb in range(4):
                        b1, b2 = 2 + qb // 2, 4 + qb // 2
                        c = (qb % 2) * 129
                        em.op("dve", lambda e, b1=b1, b2=b2, c=c: e.reciprocal(out=rs[:, 0:1], in_=self.ps[b1][:, c + 128:c + 129]),
                              reads=[f"ps{b1}"], writes=["d_rs0"])
                        em.op("dve", lambda e, b2=b2, c=c: e.reciprocal(out=rs[:, 1:2], in_=self.ps[b2][:, c + 128:c + 129]),
                              reads=[f"ps{b2}"], writes=["d_rs1"])
                        em.op("dve", lambda e: e.tensor_tensor(out=rs[:, 2:3], in0=rs[:, 1:2], in1=lam[:, 0:1], op=ALU.mult),
                              reads=["d_rs1", "d_lam"], writes=["d_rs2"])
                        em.op("act", lambda e, b1=b1, c=c: e.activation(out=o1[:], in_=self.ps[b1][:, c:c + 128], func=AF.Identity, scale=rs[:, 0:1]),
                              reads=[f"ps{b1}", "d_rs0"], writes=["d_o1"])
                        em.op("dve", lambda e, b2=b2, c=c: e.scalar_tensor_tensor(out=o1[:], in0=self.ps[b2][:, c:c + 128], scalar=rs[:, 2:3],
                                                                                 in1=o1[:], op0=ALU.mult, op1=ALU.add),
                              reads=[f"ps{b2}", "d_rs2", "d_o1"], writes=["d_o1"])
                        em.op("dve", lambda e: e.memset(rs[:, 3:4], 0.0), writes=["d_rs3"])
                        em.op("act", lambda e: e.activation(out=o2[:], in_=o1[:], func=AF.Square, accum_out=rs[:, 3:4]),
                              reads=["d_o1", "d_rs3"], writes=["d_o2", "d_rs3"])
                        em.op("act", lambda e: e.activation(out=rs[:, 4:5], in_=rs[:, 3:4], func=AF.Sqrt, scale=1.0 / 128, bias=self.epsT[:, 0:1]),
                              reads=["d_rs3", "epsT"], writes=["d_rs4"])
                        em.op("dve", lambda e: e.reciprocal(out=rs[:, 5:6], in_=rs[:, 4:5]), reads=["d_rs4"], writes=["d_rs5"])
                        em.op("dve", lambda e, qb=qb, o_=o_: e.scalar_tensor_tensor(out=o_[:, qb, :], in0=o1[:], scalar=rs[:, 5:6], in1=subg[:],
                                                                                   op0=ALU.mult, op1=ALU.mult),
                              reads=["d_o1", "d_rs5", "d_subg"], writes=[ok])
                    em.dma("sp", att[qi * 512:(qi + 1) * 512, h * 128:(h + 1) * 128].rearrange("(b p) e -> p b e", p=128), o_[:],
                           reads=[ok], writes=[("att", qi, h)])

    def attn_nsa(self, l, j, nqT, ksT, kwT, vsd, vwd, kcTr, vcTr, gat, att):
        nc, em = self.nc, self.em
        allk = lambda name: [(name, tb) for tb in range(NT)]
        with self.scope() as eso:
            kcT = self.sb(eso, "n_kcT", [128, 2, 512], BF16)
            vce = self.sb(eso, "n_vce", [128, 4, 2, 193], BF16)
            with self.scope() as es:
                z = [self.sb(es, f"n_z{i}", [128, S], BF16) for i in range(2)]
                wst = self.sb(es, "n_wst", [128, 32, 256], F32)
                w1 = self.sb(es, "n_w1", [128, 32, 256], BF16)
                w2s = self.sb(es, "n_w2s", [128, 2, 64], F32)
                w2 = self.sb(es, "n_w2", [128, 2, 64], BF16)
                posT = self.sb(es, "n_posT", [128, 32], BF16)
                pb = self.sb(es, "n_pb", [128, 2], F32)
                xg = self.sb(es, "n_xg", [128, 512], F32)
                tg = self.sb(es, "n_tg", [128, 512], F32)
                hid = self.sb(es, "n_hid", [128, 2, 512], BF16)
                kst = self.sb(es, "n_kst", [128, 24], F32)
                kd = self.sb(es, "n_kd", [128, 128], F32)
                ksq = self.sb(es, "n_ksq", [128, 64], F32)
                g64 = self.sb(es, "n_g64", [128, 64], F32)
                grow = self.sb(es, "n_grow", [1, 64], F32)
                ovl = self.sb(es, "n_ovl", [128, 4, 128], BF16)
                em.dma("sp", z[0][:], kcTr[:, :], reads=allk("kcTr"), writes=["n_z0"])
                em.dma("sp", z[1][:], vcTr[:, :], reads=allk("vcTr"), writes=["n_z1"])
                em.dma("sp", ovl[:], self.din["ovl"][:, :, :], writes=["n_ovl"])
                em.dma("sp", grow[:], self.nsa_k_gain[j, 0:1, :], writes=["n_grow"])
                self.bcast_row(g64[:], grow[0:1, :], 64, "n_g64", "n_grow")
                em.op("dve", lambda e: e.memset(hid[:], 0.0), writes=["n_hid"])
                for jj in range(2):
                    wsrc = self.nsa_cmp_w1[j, jj].rearrange("(l d) m -> d l m", d=64)
                    em.dma("sp", wst[0:64], wsrc, writes=["n_wst"])
                    em.dma("sp", wst[64:128], wsrc, writes=["n_wst2"])
                    em.op("dve", lambda e: e.tensor_copy(out=w1[:, 0:16, :], in_=wst[:, 0:16, :]), reads=["n_wst", "n_wst2"], writes=["n_w1"])
                    em.op("pool", lambda e: e.tensor_copy(out=w1[:, 16:32, :], in_=wst[:, 16:32, :]), reads=["n_wst", "n_wst2"], writes=["n_w1"])
                    em.dma("sp", w2s[:], self.nsa_cmp_w2[j, jj].rearrange("(mc p) d -> p mc d", p=128), writes=["n_w2s"])
                    em.op("dve", lambda e: e.tensor_copy(out=w2[:], in_=w2s[:]), reads=["n_w2s"], writes=["n_w2"])
                    em.dma("sp", self.tstage[0:32, 0:64], self.nsa_cmp_pos[j, jj], writes=["tstage"])
                    em.dma("sp", self.tstage[0:32, 64:128], self.nsa_cmp_pos[j, jj], writes=["tstage2"])
                    em.op("pe", lambda e: e.transpose(out=self.ps[7][:, 0:32], in_=self.tstage[0:32, 0:128], identity=self.ident[0:32, 0:32]),
                          reads=["tstage", "tstage2", "ident"], writes=["ps7"])
                    em.op("dve", lambda e: e.tensor_copy(out=posT[:], in_=self.ps[7][:, 0:32]), reads=["ps7"], writes=["n_posT"])

                    def mpb(e):
                        for mc in range(2):
                            for li in range(32):
                                r = e.matmul(self.ps[6][:, mc:mc + 1], lhsT=w1[0:64, li, mc * 128:(mc + 1) * 128], rhs=posT[0:64, li:li + 1],
                                             start=(li == 0), stop=(li == 31))
                        return r
                    em.op("pe", mpb, reads=["n_w1", "n_posT"], writes=["ps6"])
                    em.op("dve", lambda e: e.tensor_copy(out=pb[:], in_=self.ps[6][:, 0:2]), reads=["ps6"], writes=["n_pb"])
                    for kvh in range(2):
                        base = 64 * kvh
                        for mc in range(2):
                            def mh(e, mc=mc, base=base):
                                for li in range(32):
                                    r = e.matmul(self.ps[mc][:, 0:511], lhsT=w1[base:base + 64, li, mc * 128:(mc + 1) * 128],
                                                 rhs=z[jj][base:base + 64, li:li + 16 * 510 + 1:16], start=(li == 0), stop=(li == 31))
                                return r
                            em.op("pe", mh, reads=["n_w1", f"n_z{jj}"], writes=[f"ps{mc}"])
                            em.op("act", lambda e, mc=mc: e.activation(out=xg[:, 0:511], in_=self.ps[mc][:, 0:511], func=AF.Identity, bias=pb[:, mc:mc + 1], scale=1.0),
                                  reads=[f"ps{mc}", "n_pb"], writes=["n_xg"])
                            em.op("dve", lambda e: e.tensor_tensor(out=tg[:, 0:511], in0=xg[:, 0:511], in1=xg[:, 0:511], op=ALU.mult), reads=["n_xg"], writes=["n_tg"])
                            em.op("dve", lambda e: e.tensor_scalar(out=tg[:, 0:511], in0=tg[:, 0:511], scalar1=0.044715, scalar2=1.0, op0=ALU.mult, op1=ALU.add),
                                  reads=["n_tg"], writes=["n_tg"])
                            em.op("dve", lambda e: e.tensor_tensor(out=tg[:, 0:511], in0=tg[:, 0:511], in1=xg[:, 0:511], op=ALU.mult), reads=["n_tg", "n_xg"], writes=["n_tg"])
                            em.op("act", lambda e: e.activation(out=tg[:, 0:511], in_=tg[:, 0:511], func=AF.Tanh, scale=0.7978845608028654), reads=["n_tg"], writes=["n_tg"])
                            em.op("dve", lambda e: e.scalar_tensor_tensor(out=tg[:, 0:511], in0=tg[:, 0:511], scalar=1.0, in1=xg[:, 0:511], op0=ALU.add, op1=ALU.mult),
                                  reads=["n_tg", "n_xg"], writes=["n_tg"])
                            em.op("act", lambda e, mc=mc: e.activation(out=hid[:, mc, 0:511], in_=tg[:, 0:511], func=AF.Copy, scale=0.5), reads=["n_tg"], writes=["n_hid"])
                        for ct in range(4):
                            def m2(e, ct=ct):
                                for mc in range(2):
                                    r = e.matmul(self.ps[2][:, 0:64], lhsT=hid[:, mc, ct * 128:(ct + 1) * 128], rhs=w2[:, mc, :], start=(mc == 0), stop=(mc == 1))
                                return r
                            em.op("pe", m2, reads=["n_hid", "n_w2"], writes=["ps2"])
                            if jj == 1:
                                em.op("act", lambda e, ct=ct, kvh=kvh: e.copy(out=vce[:, ct, kvh, 0:64], in_=self.ps[2][:, 0:64]), reads=["ps2"], writes=["n_vce"])
                                em.op("dve", lambda e, ct=ct, kvh=kvh: e.memset(vce[:, ct, kvh, 64:65], 1.0), writes=["n_vce"])
                                em.op("dve", lambda e, ct=ct, kvh=kvh: e.tensor_copy(out=vce[:, ct, kvh, 65:193], in_=ovl[:, ct, :]), reads=["n_ovl"], writes=["n_vce"])
                            else:
                                em.op("dve", lambda e: e.memset(kst[:, 0:1], 0.0), writes=["n_kst0"])
                                em.op("act", lambda e: e.activation(out=ksq[:], in_=self.ps[2][:, 0:64], func=AF.Square, accum_out=kst[:, 0:1]),
                                      reads=["ps2", "n_kst0"], writes=["n_ksq", "n_kst0"])
                                em.op("act", lambda e: e.activation(out=kst[:, 1:2], in_=kst[:, 0:1], func=AF.Sqrt, scale=1.0 / 64, bias=self.epsT[:, 0:1]),
                                      reads=["n_kst0", "epsT"], writes=["n_kst1"])
                                em.op("dve", lambda e: e.reciprocal(out=kst[:, 2:3], in_=kst[:, 1:2]), reads=["n_kst1"], writes=["n_kst2"])
                                for r_ in range(2):
                                    em.op("dve", lambda e, r_=r_: e.scalar_tensor_tensor(out=kd[:, r_ * 64:(r_ + 1) * 64], in0=self.ps[2][:, 0:64], scalar=kst[:, 2:3],
                                                                                         in1=g64[:], op0=ALU.mult, op1=ALU.mult),
                                          reads=["ps2", "n_kst2", "n_g64"], writes=["n_kd"])
                                em.op("pe", lambda e: e.transpose(out=self.ps[3][:, 0:128], in_=kd[:], identity=self.ident[:]), reads=["n_kd", "ident"], writes=["ps3"])
                                em.op("act", lambda e, ct=ct, kvh=kvh: e.copy(out=kcT[:, kvh, ct * 128:(ct + 1) * 128], in_=self.ps[3][:, 0:128]),
                                      reads=["ps3"], writes=["n_kcT"])
            for kvh in range(2):
                with self.scope() as es:
                    nq = self.sb(es, "n_nq", [128, 2, S], BF16)
                    ks = self.sb(es, "n_ks", [128, S], BF16)
                    kw = self.sb(es, "n_kw", [128, S], BF16)
                    vs = self.sb(es, "n_vs", [128, NT, 65], BF16)
                    vw = self.sb(es, "n_vw", [128, NT, 65], BF16)
                    ebig = self.sb(es, "n_ebig", [128, S], BF16)
                    cmk = self.sb(es, "n_cmk", [128, 5, 512], BF16)
                    wmk = self.sb(es, "n_wmk", [128, 8, 512], BF16)
                    tri = self.sb(es, "n_tri", [128, 128], BF16)
                    P = [self.sb(es, f"n_P{i}", [128, 512], BF16) for i in range(2)]
                    gt = self.sb(es, "n_gt", [128, 4, 24], F32)
                    imp = self.sb(es, "n_imp", [128, 4, 128], F32)
                    score = self.sb(es, "n_score", [128, 4, 128], F32)
                    work = self.sb(es, "n_work", [128, 4, 128], F32)
                    keep = self.sb(es, "n_keep", [128, 4, 128], F32)
                    addm = self.sb(es, "n_addm", [128, 4, 128], F32)
                    m8 = self.sb(es, "n_m8", [128, 16], F32)
                    nselT = self.sb(es, "n_nselT", [128, 512], BF16)
                    oacc = self.sb(es, "n_oacc", [128, 4, 4, 64], F32)
                    rs = self.sb(es, "n_rs", [128, 4], F32)
                    em.dma("sp", nq[:, 0, :], nqT[2 * kvh], reads=allk("nqT"), writes=["n_nq"])
                    em.dma("sp", nq[:, 1, :], nqT[2 * kvh + 1], reads=allk("nqT"), writes=["n_nq1"])
                    em.dma("sp", ks[:], ksT[kvh], reads=allk("a_sks"), writes=["n_ks"])
                    em.dma("sp", kw[:], kwT[kvh], reads=allk("a_skw"), writes=["n_kw"])
                    em.dma("sp", vs[:, :, 0:64], vsd[kvh].rearrange("(n p) d -> p n d", p=128), reads=allk("vsd"), writes=["n_vs"])
                    em.dma("sp", vw[:, :, 0:64], vwd[kvh].rearrange("(n p) d -> p n d", p=128), reads=allk("vwd"), writes=["n_vw"])
                    em.op("dve", lambda e: e.memset(vs[:, :, 64:65], 1.0), writes=["n_vs1"])
                    em.op("dve", lambda e: e.memset(vw[:, :, 64:65], 1.0), writes=["n_vw1"])
                    em.dma("sp", ebig[:], self.din["ebig"][:, :], writes=["n_ebig"])
                    em.dma("sp", cmk[:], self.din["cmpmask"].rearrange("a p q -> p a q"), writes=["n_cmk"])
                    em.dma("sp", wmk[:], self.din["winmask"].rearrange("a p q -> p a q"), writes=["n_wmk"])
                    em.dma("sp", tri[:], self.din["trimask"][:, :], writes=["n_tri"])
                    qkeys = ["n_nq", "n_nq1"]
                    for qi in range(self.opts.get("nqt", S // 512)):
                        em.dma("sp", gt[:], gat[qi * 512:(qi + 1) * 512, :].rearrange("(b p) c -> p b c", p=128),
                               reads=[("gat", tb) for tb in range(4 * qi, 4 * qi + 4)], writes=["n_gt"])
                        em.dma("sp", keep[:], self.din["selkeep"][4 * qi:4 * qi + 4].rearrange("b p n -> p b n"), writes=["n_keep"])
                        em.dma("sp", addm[:], self.din["seladd"][4 * qi:4 * qi + 4].rearrange("b p n -> p b n"), writes=["n_addm"])

                        def fin(bank, per_bank, dvp, g, br, first):
                            hq = 4 * kvh + g
                            for qb in range(4):
                                bk = bank + qb // per_bank
                                c = (qb % per_bank) * dvp
                                em.op("dve", lambda e, bk=bk, c=c: e.tensor_scalar_max(out=rs[:, 0:1], in0=self.ps[bk][:, c + 64:c + 65], scalar1=1e-30),
                                      reads=[f"ps{bk}"], writes=["n_rs0"])
                                em.op("dve", lambda e: e.reciprocal(out=rs[:, 1:2], in_=rs[:, 0:1]), reads=["n_rs0"], writes=["n_rs1"])
                                em.op("dve", lambda e, qb=qb, hq=hq, br=br: e.tensor_tensor(out=rs[:, 2:3], in0=rs[:, 1:2], in1=gt[:, qb, hq * 3 + br:hq * 3 + br + 1], op=ALU.mult),
                                      reads=["n_rs1", "n_gt"], writes=["n_rs2"])
                                if first:
                                    em.op("act", lambda e, bk=bk, c=c, g=g, qb=qb: e.activation(out=oacc[:, g, qb, :], in_=self.ps[bk][:, c:c + 64], func=AF.Identity, scale=rs[:, 2:3]),
                                          reads=[f"ps{bk}", "n_rs2"], writes=[("n_oacc", g)])
                                    if g == 0:
                                        em.op("dve", lambda e, bk=bk, c=c, qb=qb: e.tensor_single_scalar(out=imp[:, qb, :], in_=self.ps[bk][:, c + 65:c + 193], scalar=rs[:, 1:2], op=ALU.mult),
                                              reads=[f"ps{bk}", "n_rs1"], writes=["n_imp"])
                                    else:
                                        em.op("dve", lambda e, bk=bk, c=c, qb=qb: e.scalar_tensor_tensor(out=imp[:, qb, :], in0=self.ps[bk][:, c + 65:c + 193], scalar=rs[:, 1:2], in1=imp[:, qb, :],
                                                                                                         op0=ALU.mult, op1=ALU.add),
                                              reads=[f"ps{bk}", "n_rs1", "n_imp"], writes=["n_imp"])
                                else:
                                    em.op("dve", lambda e, bk=bk, c=c, g=g, qb=qb: e.scalar_tensor_tensor(out=oacc[:, g, qb, :], in0=self.ps[bk][:, c:c + 64], scalar=rs[:, 2:3], in1=oacc[:, g, qb, :],
                                                                                                           op0=ALU.mult, op1=ALU.add),
                                          reads=[f"ps{bk}", "n_rs2", ("n_oacc", g)], writes=[("n_oacc", g)])

                        def qap(g):
                            hq = 4 * kvh + g
                            r_ = hq % 2
                            return nq[64 * r_:64 * r_ + 64, g // 2, qi * 512:(qi + 1) * 512], r_

                        for g in range(4):
                            q_ap, r_ = qap(g)
                            items = []
                            for jc in range(4):
                                dl = 512 * qi - 2048 * jc
                                if dl <= -512:
                                    continue
                                masks = [] if dl >= 2560 else [(self.identb[:], cmk[:, dl // 512, :], 0, 512, ["identb", "n_cmk"])]
                                items.append(dict(kT=kcT[64 * r_:64 * r_ + 64, kvh, jc * 128:(jc + 1) * 128], kkey="n_kcT", v=vce[:, jc, kvh, :], vkey="n_vce",
                                                  qlo=0, qhi=512, masks=masks))
                            self.att_tiles(q_ap, qkeys[g // 2], items, 193, (2, 3), 2, P, "n_")
                            fin(2, 2, 193, g, 0, True)
                        em.op("dve", lambda e: e.tensor_tensor(out=score[:], in0=imp[:], in1=keep[:], op=ALU.mult), reads=["n_imp", "n_keep"], writes=["n_score"])
                        em.op("dve", lambda e: e.tensor_tensor(out=score[:], in0=score[:], in1=addm[:], op=ALU.add), reads=["n_score", "n_addm"], writes=["n_score"])
                        for qb in range(4):
                            em.op("dve", lambda e, qb=qb: e.max(out=m8[:, 0:8], in_=score[:, qb, :]), reads=["n_score"], writes=["n_m8a"])
                            em.op("dve", lambda e, qb=qb: e.match_replace(out=work[:, qb, :], in_to_replace=m8[:, 0:8], in_values=score[:, qb, :], imm_value=-3.0e9),
                                  reads=["n_score", "n_m8a"], writes=["n_work"])
                            em.op("dve", lambda e, qb=qb: e.max(out=m8[:, 8:16], in_=work[:, qb, :]), reads=["n_work"], writes=["n_m8b"])
                            em.op("dve", lambda e, qb=qb: e.tensor_scalar(out=work[:, qb, :], in0=score[:, qb, :], scalar1=m8[:, 15:16], scalar2=1.0, op0=ALU.is_ge, op1=ALU.subtract),
                                  reads=["n_score", "n_m8b", "n_work"], writes=["n_work"])
                            em.op("pe", lambda e, qb=qb: e.transpose(out=self.ps[6][:, qb * 128:(qb + 1) * 128], in_=work[:, qb, :], identity=self.ident[:]),
                                  reads=["n_work", "ident"], writes=["ps6"])
                        em.op("act", lambda e: e.activation(out=nselT[:], in_=self.ps[6][:, :], func=AF.Copy, scale=BIG), reads=["ps6"], writes=["n_nselT"])
                        for g in range(4):
                            q_ap, r_ = qap(g)
                            items = []
                            for kj in range(4 * qi + 4):
                                r = kj - 4 * qi
                                qlo = max(0, r) * 128
                                masks = [(ebig[:, kj * 128:(kj + 1) * 128], nselT[:, qlo:512], qlo, 512, ["n_ebig", "n_nselT"])]
                                if r >= 0:
                                    masks.append((self.identb[:], tri[:], r * 128, (r + 1) * 128, ["identb", "n_tri"]))
                                items.append(dict(kT=ks[64 * r_:64 * r_ + 64, kj * 128:(kj + 1) * 128], kkey="n_ks", v=vs[:, kj, :], vkey="n_vs",
                                                  qlo=qlo, qhi=512, masks=masks))
                            self.att_tiles(q_ap, qkeys[g // 2], items, 65, (4,), 4, P, "n_")
                            fin(4, 4, 65, g, 1, False)
                            items = []
                            for kj in range(max(0, 4 * qi - 4), 4 * qi + 4):
                                r8 = kj - 4 * qi + 4
                                qlo = max(0, r8 - 4) * 128
                                qhi = (min(3, r8) + 1) * 128
                                items.append(dict(kT=kw[64 * r_:64 * r_ + 64, kj * 128:(kj + 1) * 128], kkey="n_kw", v=vw[:, kj, :], vkey="n_vw",
                                                  qlo=qlo, qhi=qhi, masks=[(self.identb[:], wmk[:, r8, qlo:qhi], qlo, qhi, ["identb", "n_wmk"])]))
                            self.att_tiles(q_ap, qkeys[g // 2], items, 65, (5,), 4, P, "n_")
                            fin(5, 4, 65, g, 2, False)
                        for g in range(4):
                            c0 = 512 + (4 * kvh + g) * 64
                            em.dma("sp", att[qi * 512:(qi + 1) * 512, c0:c0 + 64].rearrange("(b p) d -> p b d", p=128),
                                   oacc[:, g, :, :], reads=[("n_oacc", g)], writes=[("attn", qi, kvh, g)])

    def attn_out(self, l, j, src, att):
        nc, em = self.nc, self.em
        with self.scope() as es:
            wo = self.sb(es, "o_w", [128, 8, D], BF16)
            stg = [self.sb(es, f"o_stg{i}", [128, D], F32) for i in range(2)]
            at = [self.sb(es, f"o_at{i}", [128, D], F32) for i in range(2)]
            xt = [self.sb(es, f"o_xt{i}", [128, D], F32) for i in range(2)]
            aT = self.sb(es, "o_aT", [128, 8, 128], BF16)
            wv = self.attn_w_out[j].rearrange("(k p) n -> k p n", p=128)
            for k in range(8):
                em.dma("sp", stg[k % 2][:], wv[k], writes=[f"o_stg{k % 2}"])
                em.op("dve" if k % 2 == 0 else "pool", lambda e, k=k: e.tensor_tensor(
                    out=wo[:, k, :], in0=stg[k % 2][:], in1=self.gbc[:], op=ALU.mult),
                    reads=[f"o_stg{k % 2}", "gbc"], writes=["o_w"])
            for tb in range(self.opts.get("ntb", NT)):
                t0 = tb * 128
                a_, x_ = at[tb % 2], xt[tb % 2]
                ak, xk = f"o_at{tb % 2}", f"o_xt{tb % 2}"
                em.dma("sp", a_[:], att[t0:t0 + 128, :], reads=[("att", t0 // 512, h) for h in range(4)] + [("attn", t0 // 512, kv, g) for kv in range(2) for g in range(4)],
                       writes=[ak])
                em.dma("sp", x_[:], src[t0:t0 + 128, :], reads=[("xs", t0 // 512)], writes=[xk])
                for h in range(2):
                    def tr(e, h=h):
                        for k in range(4):
                            r = e.transpose(out=self.ps[h][:, k * 128:(k + 1) * 128], in_=a_[:, (h * 4 + k) * 128:(h * 4 + k + 1) * 128],
                                            identity=self.ident[:])
                        return r
                    em.op("pe", tr, reads=[ak, "ident"], writes=[f"ps{h}"])
                    em.op("act" if h == 0 else "dve", (lambda e, h=h: e.copy(out=aT[:, h * 4:(h + 1) * 4, :].rearrange("p k t -> p (k t)"), in_=self.ps[h][:, :]))
                          if h == 0 else (lambda e, h=h: e.tensor_copy(out=aT[:, h * 4:(h + 1) * 4, :].rearrange("p k t -> p (k t)"), in_=self.ps[h][:, :])),
                          reads=[f"ps{h}"], writes=["o_aT"])
                for h in range(2):
                    po = 2 + h + 2 * (tb % 2)

                    def mmo(e, h=h, po=po):
                        for k in range(8):
                            r = e.matmul(self.ps[po][:, :], lhsT=aT[:, k, :], rhs=wo[:, k, h * 512:(h + 1) * 512], start=(k == 0), stop=(k == 7))
                        return r
                    em.op("pe", mmo, reads=["o_aT", "o_w"], writes=[f"ps{po}"])
                    em.op("dve", lambda e, h=h, po=po: e.tensor_tensor(out=x_[:, h * 512:(h + 1) * 512], in0=self.ps[po][:, :],
                                                                       in1=x_[:, h * 512:(h + 1) * 512], op=ALU.add),
                          reads=[f"ps{po}", xk], writes=[xk])
                em.dma("sp", self.y[t0:t0 + 128, :], x_[:], reads=[xk], writes=[("xs", t0 // 512)])


def _consts():
    bf = ml_dtypes.bfloat16
    kl = np.arange(128)[:, None]
    ql = np.arange(128)[None, :]
    tri = np.where(ql >= kl, 0.0, -BIG).astype(bf)
    tl = np.arange(512)[None, :]
    cmpmask = np.stack([np.where(16 * kl + 31 <= 512 * a + tl, 0.0, -BIG) for a in range(5)]).astype(bf)
    winmask = np.stack([np.where(((tl - kl - 128 * (r8 - 4)) >= 0) & ((tl - kl - 128 * (r8 - 4)) < 512), 0.0, -BIG)
                        for r8 in range(8)]).astype(bf)
    ebig = (np.arange(128)[:, None] == (np.arange(S)[None, :] // 64)).astype(bf)
    c = np.arange(512)[:, None]
    n = np.arange(128)[None, :]
    ov = ((16 * c < 64 * n + 64) & (16 * c + 32 > 64 * n) & (c < 511)).astype(np.float32)
    ovl = ov.reshape(4, 128, 128).transpose(1, 0, 2).astype(bf)
    t = np.arange(S)[:, None]
    cur = t // 64
    nn = np.arange(128)[None, :]
    valid = nn <= cur
    f0, f1, f2 = (nn == 0), (nn == cur), (nn == cur - 1)
    forced = f0 | f1 | f2
    keep = (valid & ~forced).astype(np.float32)
    add = np.where(f0, 10002.0, np.where(f1, 10001.0, np.where(f2, 10000.0, np.where(valid, 0.0, -1.0e9)))).astype(np.float32)
    return {"ident": np.eye(128, dtype=np.float32), "trimask": tri, "cmpmask": cmpmask, "winmask": winmask, "ebig": ebig,
            "ovl": np.ascontiguousarray(ovl), "selkeep": keep.reshape(64, 128, 128), "seladd": add.reshape(64, 128, 128)}


def _run(plan, inputs, n_cores=8, dbg=(), xs=None, trace=False, opts=None):
    prog = Prog(plan, dbg, opts, ncores=n_cores)
    nc = prog.build()
    print("built: instructions", prog.em.ninst, "sems", prog.em.nsem, flush=True)
    consts = _consts()
    in_maps = []
    for b in range(n_cores):
        m = {}
        for name in prog.din:
            if name == "x":
                m[name] = np.ascontiguousarray(inputs["x"][b] if xs is None else xs[b])
            elif name == "c":
                m[name] = np.ascontiguousarray(inputs["c"][b:b + 1])
            elif name in consts:
                m[name] = consts[name]
            elif name in prog.lsel:
                sl = prog.lsel[name]
                arr = inputs[name].reshape(2, 31, D) if name == "conv_dw_w" else inputs[name]
                m[name] = np.ascontiguousarray(arr[sl:sl + 1])
            elif name in ("moe_w1", "moe_w2") and prog.nr > 1:
                m[name] = np.ascontiguousarray(inputs[name][:, 4 * b:4 * b + 4])
            elif name == "conv_dw_w":
                m[name] = np.ascontiguousarray(inputs[name].reshape(2, 31, D))
            else:
                m[name] = np.ascontiguousarray(inputs[name])
        in_maps.append(m)
    res = run_bass_kernel_spmd(nc, in_maps, core_ids=list(range(n_cores)), trace=trace)
    return res


FULL_PLAN = [(k, l) for l in range(DEPTH) for k in ("mix", "ffn")]


def kernel(**inputs):
    xs = [np.ascontiguousarray(inputs["x"][b]) for b in range(8)]
    launches = [[("mix", 0)], [("ffn", 0)], [("mix", 1), ("ffn", 1)], [("mix", 2)], [("ffn", 2)], [("mix", 3), ("ffn", 3)]]
    for steps in launches:
        res = _run(steps, inputs, n_cores=8, xs=xs)
        xs = [np.asarray(r["y"], dtype=np.float32) for r in res.results]
    return np.stack(xs, axis=0).astype(np.float32)
```
